# Optimizing a Trainium2 kernel written in Bass

```python
import jax
import jax.numpy as jnp
from jax import lax
import numpy as np

D_MODEL = 1024
BATCH = 4
SEQ = 4096
DEPTH = 2

CONV_DIM = 512
CONV_KERNEL = 3
GMLP_DIM = 512
GMLP_GROUPS = 8
GMLP_GROUP_DIM = GMLP_DIM // GMLP_GROUPS
GMLP_CHUNK = 128
MIX_IN_EVEN = 3 * CONV_DIM + 2 * GMLP_DIM
MIX_OUT_EVEN = CONV_DIM + GMLP_DIM
N_HEADS = 8
HEAD_DIM = 128
ROT_DIM = HEAD_DIM // 4
ROPE_THETA = 500000.0
MOBA_BLOCK = 256
MOBA_TOPK = 3
MOBA_Q_CHUNK = 64
D_FF_DENSE = 2816
N_EXPERTS = 8
TOP_K_EXPERTS = 2
D_FF_EXPERT = 3584
EPS = 1e-6
N_EVEN = (DEPTH + 1) // 2
N_ODD = DEPTH // 2

kernel_name = 'hybrid_conv_gmlp_moba_moe_block'


def rms_norm(x, g):
    xf = x.astype(jnp.float32)
    y = xf * lax.rsqrt(jnp.mean(jnp.square(xf), axis=-1, keepdims=True) + EPS)
    return (y * g.astype(jnp.float32)).astype(x.dtype)


def layer_norm(x, g, b):
    xf = x.astype(jnp.float32)
    mu = jnp.mean(xf, axis=-1, keepdims=True)
    var = jnp.mean(jnp.square(xf - mu), axis=-1, keepdims=True)
    y = (xf - mu) * lax.rsqrt(var + EPS) * g.astype(jnp.float32) + b.astype(jnp.float32)
    return y.astype(x.dtype)


def rope_partial(x):
    seq = x.shape[1]
    half = ROT_DIM // 2
    inv_freq = ROPE_THETA ** (-jnp.arange(half, dtype=jnp.float32) / half)
    ang = jnp.arange(seq, dtype=jnp.float32)[:, None] * inv_freq[None, :]
    cos = jnp.cos(ang)[:, None, :]
    sin = jnp.sin(ang)[:, None, :]
    xf = x.astype(jnp.float32)
    x1 = xf[..., :half]
    x2 = xf[..., half:ROT_DIM]
    out = jnp.concatenate([x1 * cos - x2 * sin, x2 * cos + x1 * sin, xf[..., ROT_DIM:]], axis=-1)
    return out.astype(x.dtype)


def conv_gmlp_mixer(h, w_in, conv_w, ln_g, ln_b, w_s, b_s, w_out):
    bsz, seq, _ = h.shape
    proj = h @ w_in
    a_h, a_c, a_b, g_u, g_v = jnp.split(
        proj, [CONV_DIM, 2 * CONV_DIM, 3 * CONV_DIM, 3 * CONV_DIM + GMLP_DIM], axis=-1)
    z = a_c * a_h
    z = lax.conv_general_dilated(
        z, conv_w[:, None, :], window_strides=(1,), padding=[(CONV_KERNEL - 1, 0)],
        dimension_numbers=('NWC', 'WIO', 'NWC'), feature_group_count=CONV_DIM)
    y_a = a_b * z
    v = layer_norm(g_v, ln_g, ln_b)
    n_chunks = seq // GMLP_CHUNK
    v = v.reshape(bsz, n_chunks, GMLP_CHUNK, GMLP_GROUPS, GMLP_GROUP_DIM)
    causal = jnp.tril(jnp.ones((GMLP_CHUNK, GMLP_CHUNK), dtype=bool))
    w_causal = jnp.where(causal, w_s, jnp.zeros((), w_s.dtype))
    mixed = jnp.einsum('gts,bcsgd->bctgd', w_causal, v) + jnp.transpose(b_s)[:, :, None]
    y_b = g_u * mixed.reshape(bsz, seq, GMLP_DIM)
    return jnp.concatenate([y_a, y_b], axis=-1) @ w_out


def moba_attention(q, k, v):
    bsz, n_heads, seq, hd = q.shape
    n_blocks = -(-seq // MOBA_BLOCK)
    pad = n_blocks * MOBA_BLOCK - seq
    widths = ((0, 0), (0, 0), (0, pad), (0, 0))
    kb = jnp.pad(k, widths).reshape(bsz, n_heads, n_blocks, MOBA_BLOCK, hd)
    vb = jnp.pad(v, widths).reshape(bsz, n_heads, n_blocks, MOBA_BLOCK, hd)
    k_mean = jnp.mean(kb.astype(jnp.float32), axis=3)
    gate = jnp.einsum('bhsd,bhnd->bhsn', q.astype(jnp.float32), k_mean)
    q_block = jnp.arange(seq) // MOBA_BLOCK
    past = jnp.arange(n_blocks)[None, :] < q_block[:, None]
    gate = jnp.where(past, gate, -jnp.inf)
    n_sel = min(MOBA_TOPK, n_blocks)
    _, sel_idx = lax.top_k(gate, n_sel)
    sel_valid = sel_idx < q_block[:, None]
    n_chunks = seq // MOBA_Q_CHUNK

    def to_chunks(a):
        a = a.reshape(bsz, n_heads, n_chunks, MOBA_Q_CHUNK, *a.shape[3:])
        return jnp.moveaxis(a, 2, 0)

    starts = jnp.arange(n_chunks) * MOBA_Q_CHUNK
    b_ix = jnp.arange(bsz)[:, None, None, None]
    h_ix = jnp.arange(n_heads)[None, :, None, None]
    scale = hd ** -0.5

    def attend_chunk(args):
        q_c, idx_c, valid_c, start = args
        blk = start // MOBA_BLOCK
        k_own = lax.dynamic_index_in_dim(kb, blk, axis=2, keepdims=False)
        v_own = lax.dynamic_index_in_dim(vb, blk, axis=2, keepdims=False)
        k_sel = kb[b_ix, h_ix, idx_c]
        v_sel = vb[b_ix, h_ix, idx_c]
        s_sel = jnp.einsum('bhqd,bhqnld->bhqnl', q_c, k_sel).reshape(
            bsz, n_heads, MOBA_Q_CHUNK, n_sel * MOBA_BLOCK)
        s_own = jnp.einsum('bhqd,bhld->bhql', q_c, k_own)
        q_pos = start + jnp.arange(MOBA_Q_CHUNK)
        k_pos = blk * MOBA_BLOCK + jnp.arange(MOBA_BLOCK)
        sel_mask = jnp.repeat(valid_c, MOBA_BLOCK, axis=-1)
        own_mask = k_pos[None, :] <= q_pos[:, None]
        s = jnp.concatenate([
            jnp.where(sel_mask, s_sel.astype(jnp.float32) * scale, -jnp.inf),
            jnp.where(own_mask, s_own.astype(jnp.float32) * scale, -jnp.inf)], axis=-1)
        p = jax.nn.softmax(s, axis=-1).astype(v.dtype)
        p_sel = p[..., :n_sel * MOBA_BLOCK].reshape(bsz, n_heads, MOBA_Q_CHUNK, n_sel, MOBA_BLOCK)
        p_own = p[..., n_sel * MOBA_BLOCK:]
        return (jnp.einsum('bhqnl,bhqnld->bhqd', p_sel, v_sel)
                + jnp.einsum('bhql,bhld->bhqd', p_own, v_own))

    out = lax.map(attend_chunk, (to_chunks(q), to_chunks(sel_idx), to_chunks(sel_valid), starts))
    return jnp.moveaxis(out, 0, 2).reshape(bsz, n_heads, seq, hd)


def moba_mixer(h, w_qkv, q_g, k_g, w_o):
    bsz, seq, _ = h.shape
    qkv = (h @ w_qkv).reshape(bsz, seq, 3, N_HEADS, HEAD_DIM)
    q = rope_partial(rms_norm(qkv[:, :, 0], q_g))
    k = rope_partial(rms_norm(qkv[:, :, 1], k_g))
    v = qkv[:, :, 2]
    o = moba_attention(q.transpose(0, 2, 1, 3), k.transpose(0, 2, 1, 3), v.transpose(0, 2, 1, 3))
    o = o.transpose(0, 2, 1, 3).reshape(bsz, seq, N_HEADS * HEAD_DIM)
    return o @ w_o


def swiglu(h, w_gate, w_up, w_down):
    return (jax.nn.silu(h @ w_gate) * (h @ w_up)) @ w_down


def moe_swiglu(h, w_router, w_gate, w_up, w_down):
    logits = (h @ w_router).astype(jnp.float32)
    top_v, top_i = lax.top_k(logits, TOP_K_EXPERTS)
    probs = jax.nn.softmax(top_v, axis=-1)
    gates = jnp.sum(jax.nn.one_hot(top_i, N_EXPERTS, dtype=jnp.float32) * probs[..., None], axis=-2)
    gates = gates.astype(h.dtype)
    y = jnp.zeros_like(h)
    for e in range(N_EXPERTS):
        y = y + gates[..., e:e + 1] * swiglu(h, w_gate[e], w_up[e], w_down[e])
    return y


def setup_inputs(seed: int = 0) -> dict:
    key = jax.random.key(seed)
    ks = jax.random.split(key, 24)

    def nrm(k, shape, scale):
        return jax.random.normal(k, shape, jnp.float32) * scale

    def gain(k, shape):
        return 1.0 + 0.02 * jax.random.normal(k, shape, jnp.float32)

    return {
        'x': nrm(ks[0], (BATCH, SEQ, D_MODEL), 1.0),
        'e_mix_norm': gain(ks[1], (N_EVEN, D_MODEL)),
        'e_w_in': nrm(ks[2], (N_EVEN, D_MODEL, MIX_IN_EVEN), D_MODEL ** -0.5),
        'e_conv_w': nrm(ks[3], (N_EVEN, CONV_KERNEL, CONV_DIM), CONV_KERNEL ** -0.5),
        'e_gmlp_ln_g': gain(ks[4], (N_EVEN, GMLP_DIM)),
        'e_gmlp_ln_b': nrm(ks[5], (N_EVEN, GMLP_DIM), 0.02),
        'e_w_spatial': nrm(ks[6], (N_EVEN, GMLP_GROUPS, GMLP_CHUNK, GMLP_CHUNK), GMLP_CHUNK ** -0.5),
        'e_b_spatial': gain(ks[7], (N_EVEN, GMLP_GROUPS, GMLP_CHUNK)),
        'e_w_out': nrm(ks[8], (N_EVEN, MIX_OUT_EVEN, D_MODEL), MIX_OUT_EVEN ** -0.5),
        'e_ffn_norm': gain(ks[9], (N_EVEN, D_MODEL)),
        'e_w_gate': nrm(ks[10], (N_EVEN, D_MODEL, D_FF_DENSE), D_MODEL ** -0.5),
        'e_w_up': nrm(ks[11], (N_EVEN, D_MODEL, D_FF_DENSE), D_MODEL ** -0.5),
        'e_w_down': nrm(ks[12], (N_EVEN, D_FF_DENSE, D_MODEL), D_FF_DENSE ** -0.5),
        'o_mix_norm': gain(ks[13], (N_ODD, D_MODEL)),
        'o_w_qkv': nrm(ks[14], (N_ODD, D_MODEL, 3 * N_HEADS * HEAD_DIM), D_MODEL ** -0.5),
        'o_q_norm': gain(ks[15], (N_ODD, HEAD_DIM)),
        'o_k_norm': gain(ks[16], (N_ODD, HEAD_DIM)),
        'o_w_o': nrm(ks[17], (N_ODD, N_HEADS * HEAD_DIM, D_MODEL), (N_HEADS * HEAD_DIM) ** -0.5),
        'o_ffn_norm': gain(ks[18], (N_ODD, D_MODEL)),
        'o_w_router': nrm(ks[19], (N_ODD, D_MODEL, N_EXPERTS), D_MODEL ** -0.5),
        'o_w_gate': nrm(ks[20], (N_ODD, N_EXPERTS, D_MODEL, D_FF_EXPERT), D_MODEL ** -0.5),
        'o_w_up': nrm(ks[21], (N_ODD, N_EXPERTS, D_MODEL, D_FF_EXPERT), D_MODEL ** -0.5),
        'o_w_down': nrm(ks[22], (N_ODD, N_EXPERTS, D_FF_EXPERT, D_MODEL), D_FF_EXPERT ** -0.5),
    }


def reference(x, e_mix_norm, e_w_in, e_conv_w, e_gmlp_ln_g, e_gmlp_ln_b, e_w_spatial,
              e_b_spatial, e_w_out, e_ffn_norm, e_w_gate, e_w_up, e_w_down,
              o_mix_norm, o_w_qkv, o_q_norm, o_k_norm, o_w_o, o_ffn_norm, o_w_router,
              o_w_gate, o_w_up, o_w_down):
    h = x
    for layer in range(DEPTH):
        i = layer // 2
        if layer % 2 == 0:
            h = h + conv_gmlp_mixer(rms_norm(h, e_mix_norm[i]), e_w_in[i], e_conv_w[i],
                                    e_gmlp_ln_g[i], e_gmlp_ln_b[i], e_w_spatial[i],
                                    e_b_spatial[i], e_w_out[i])
            h = h + swiglu(rms_norm(h, e_ffn_norm[i]), e_w_gate[i], e_w_up[i], e_w_down[i])
        else:
            h = h + moba_mixer(rms_norm(h, o_mix_norm[i]), o_w_qkv[i], o_q_norm[i],
                               o_k_norm[i], o_w_o[i])
            h = h + moe_swiglu(rms_norm(h, o_ffn_norm[i]), o_w_router[i], o_w_gate[i],
                               o_w_up[i], o_w_down[i])
    return h
```

```python
import numpy as np
from contextlib import ExitStack
import concourse.bass as bass
import concourse.mybir as mybir
from concourse.bass_utils import run_bass_kernel_spmd

F32 = mybir.dt.float32
BF16 = mybir.dt.bfloat16
AF = mybir.ActivationFunctionType
ALU = mybir.AluOpType
AX = mybir.AxisListType

D = 1024
T = 2048
NT = 16
NG = 4
EPS = 1e-6
FF0 = 2816
FFE = 3584
NE = 8
L0_BLOCKS = [512, 512, 512, 512, 512, 256]
MOE_BLOCKS = [512] * 7
SCALE = 128 ** -0.5
NDSEM = 90
DBG_NORM = 9
SPARSE = True
NGRP = 15
NSLOT = NGRP * 512
U32 = mybir.dt.uint32
ENGS = ("pe", "act", "dve", "pool", "sp")


class Res:
    __slots__ = ("name", "last_w", "rd", "rd_dma", "last_dma", "sem", "excl")

    def __init__(self, name, excl=False):
        self.name = name
        self.sem = None
        self.excl = excl
        self.reset()

    def reset(self):
        self.last_w = None
        self.rd = {}
        self.rd_dma = []
        self.last_dma = None
        self.sem = None


class Ins:
    __slots__ = ("eng", "fn", "deps", "signal", "sem", "val", "key")

    def __init__(self, eng, fn, key):
        self.eng = eng
        self.fn = fn
        self.key = key
        self.deps = []
        self.signal = False
        self.sem = None
        self.val = 0


class Sched:
    def __init__(self, nc, es):
        self.nc = nc
        self.esem = {e: es.enter_context(nc.semaphore("e_" + e)) for e in ENGS}
        self.ecnt = {e: 0 for e in ENGS}
        self.dsems = [es.enter_context(nc.semaphore("d%d" % i)) for i in range(NDSEM)]
        self.dcnt = [0] * NDSEM
        self.waited = {e: {} for e in ENGS}
        self.allres = []
        self.n_ins = 0
        self.begin()

    def res(self, name, excl=False):
        r = Res(name, excl)
        self.allres.append(r)
        return r

    def begin(self):
        self.q = {e: [] for e in ENGS}
        self.keys = []
        self.free_sw = list(range(0, 30))
        self.free_hw = list(range(30, NDSEM))
        for r in self.allres:
            r.reset()

    def add(self, eng, fn, reads=(), writes=(), key=None):
        ins = Ins(eng, fn, key)
        deps = set()
        xr = [r for r in reads if r.excl]
        if xr:
            writes = list(writes) + xr
            reads = [r for r in reads if not r.excl]
        for r in reads:
            if r.last_w is not None:
                deps.add(r.last_w)
        for w in writes:
            if w.last_w is not None:
                deps.add(w.last_w)
            deps.update(w.rd.values())
            deps.update(w.rd_dma)
        if key is not None and key.last_dma is not None:
            deps.add(key.last_dma)
        final = []
        for d in deps:
            if d is ins:
                continue
            if d.key is None and key is None and d.eng == "pe" and eng == "pe":
                continue
            if d.key is None:
                d.signal = True
            final.append(d)
        ins.deps = final
        for r in reads:
            if key is not None:
                r.rd_dma.append(ins)
            else:
                r.rd[eng] = ins
        for w in writes:
            w.last_w = ins
            w.rd = {}
            w.rd_dma = []
        if key is not None:
            key.last_dma = ins
            if key.sem is None:
                key.sem = (self.free_sw if eng == "pool" else self.free_hw).pop()
                self.keys.append(key)
            self.dcnt[key.sem] += 16
            ins.sem = self.dsems[key.sem]
            ins.val = self.dcnt[key.sem]
        self.q[eng].append(ins)
        self.n_ins += 1
        return ins

    def end(self, extra_wait=()):
        dmas = [k.last_dma for k in self.keys if k.last_dma is not None]
        b = Ins("sp", None, None)
        b.deps = list(dmas) + list(extra_wait)
        self.q["sp"].append(b)
        for e in ENGS:
            for ins in self.q[e]:
                if ins.key is None and ins.signal:
                    self.ecnt[e] += 1
                    ins.sem = self.esem[e]
                    ins.val = self.ecnt[e]
        nc = self.nc
        sched = self

        def mk(e):
            def f(eng):
                waited = sched.waited[e]
                for ins in sched.q[e]:
                    for d in ins.deps:
                        sid = id(d.sem)
                        if waited.get(sid, -1) < d.val:
                            eng.wait_ge(d.sem, d.val)
                            waited[sid] = d.val
                    if ins.fn is None:
                        continue
                    r = ins.fn(eng)
                    if ins.key is not None:
                        r.then_inc(ins.sem, 16)
                    elif ins.signal:
                        r.then_inc(ins.sem, 1)
            return f

        _BOUND_REGS.clear()
        with nc.Block(no_gpsimd_drain=True) as block:
            block.tensor(mk("pe"))
            block.scalar(mk("act"))
            block.vector(mk("dve"))
            block.gpsimd(mk("pool"))
            block.sync(mk("sp"))
        self.begin()


class _Borrow:
    def __init__(self, es):
        self.es = es

    def __enter__(self):
        return self.es

    def __exit__(self, *a):
        return False


def i_mm(out, lhsT, rhs, start, stop, skip=False):
    if skip:
        return lambda e: e.matmul(out, lhsT, rhs, start=start, stop=stop, skip_group_check=True)
    return lambda e: e.matmul(out, lhsT, rhs, start=start, stop=stop)


def i_tr(out, in_, ident):
    return lambda e: e.transpose(out, in_, ident)


def i_act(out, in_, func, bias=None, scale=None, accum_out=None):
    kw = {}
    if bias is not None:
        kw["bias"] = bias
    if scale is not None:
        kw["scale"] = scale
    if accum_out is not None:
        kw["accum_out"] = accum_out
    return lambda e: e.activation(out=out, in_=in_, func=func, **kw)


def i_ts(out, in0, s1, s2, op0, op1=None):
    if op1 is None:
        return lambda e: e.tensor_scalar(out=out, in0=in0, scalar1=s1, scalar2=None, op0=op0)
    return lambda e: e.tensor_scalar(out=out, in0=in0, scalar1=s1, scalar2=s2, op0=op0, op1=op1)


def i_tt(out, in0, in1, op):
    return lambda e: e.tensor_tensor(out=out, in0=in0, in1=in1, op=op)


def i_stt(out, in0, scalar, in1, op0, op1):
    return lambda e: e.scalar_tensor_tensor(out=out, in0=in0, scalar=scalar, in1=in1, op0=op0, op1=op1)


def i_copy(out, in_):
    return lambda e: e.tensor_copy(out=out, in_=in_)


def i_acopy(out, in_):
    return lambda e: e.activation(out=out, in_=in_, func=AF.Identity)


def i_dma(out, in_):
    return lambda e: e.dma_start(out=out, in_=in_)


_BOUND_REGS = {}


def _bound_reg(e, bound):
    r = _BOUND_REGS.get(bound)
    if r is None:
        r = e.to_reg(bound)
        _BOUND_REGS[bound] = r
    return r


def i_gather(out, in_, idx, bound):
    return lambda e: e.indirect_dma_start(out=out, out_offset=None, in_=in_,
                                          in_offset=bass.IndirectOffsetOnAxis(ap=idx, axis=0),
                                          bounds_check=_bound_reg(e, bound), oob_is_err=False)


def i_scatter(out, idx, in_, bound):
    return lambda e: e.indirect_dma_start(out=out, out_offset=bass.IndirectOffsetOnAxis(ap=idx, axis=0),
                                          in_=in_, in_offset=None, bounds_check=_bound_reg(e, bound), oob_is_err=False)


def i_memset(ap, v):
    return lambda e: e.memset(ap, v)


def i_red(out, in_, op, axis=AX.X, absval=None):
    if absval:
        return lambda e: e.tensor_reduce(out=out, in_=in_, axis=axis, op=op, apply_absolute_value=True)
    return lambda e: e.tensor_reduce(out=out, in_=in_, axis=axis, op=op)


def i_max8(out, in_):
    return lambda e: e.max(out=out, in_=in_)


def i_bnstats(out, in_):
    return lambda e: e.bn_stats(out=out, in_=in_)


def i_bnaggr(out, in_):
    return lambda e: e.bn_aggr(out=out, in_=in_)


def i_recip(out, in_):
    return lambda e: e.reciprocal(out=out, in_=in_)


class Builder:
    def __init__(self, stop_after=None):
        self.stop_after = stop_after
        self.nc = bass.Bass("TRN2", target_bir_lowering=False)
        self.uid = 0

    def name(self, p):
        self.uid += 1
        return "%s_%d" % (p, self.uid)

    def din(self, name, shape, dt=F32):
        return self.nc.dram_tensor(name, list(shape), dt, kind="ExternalInput").ap()

    def sb(self, es, shape, dt, p="t"):
        return es.enter_context(self.nc.sbuf_tensor(self.name(p), list(shape), dt))

    def build(self):
        nc = self.nc
        shapes = {
            "xo": [NT, 128, D], "xp": [NT, 128, D], "gn": [4, 128, D], "win": [128, 8, 2560], "wout": [128, 8, D],
            "convw": [128, 12], "lng": [128, 4], "lnb": [128, 512], "wsT": [128, 8, 128], "bsp": [128, 512],
            "tril": [128, 128], "egu": [128, 8 * 2 * FF0], "edn": [128, (FF0 // 128) * D], "wqkv": [128, 8, 3072],
            "gq": [128, 128], "gk": [128, 128], "wo": [128, 8, D], "wr": [128, 8, 8],
            "mgu": ([NE * 128 * 28, 2048] if SPARSE else [NE, 128, 8 * 2 * FFE]), "mdn": ([NE * 128 * 14, 2048] if SPARSE else [NE, 128, (FFE // 128) * D]),
            "trilb": [128, 128], "giota": [128, NGRP], "basew": [128, 28], "based": [128, 14], "rope": [128, 2, 32, 16],
            "ident": [128, 128], "dmask": [128, 512], "eoh": [128, 2048], "gbias": [128, 256],
        }
        bld = self

        class LazyIn(dict):
            def __missing__(self, k):
                v = bld.din(k, shapes[k])
                self[k] = v
                return v
        I = LazyIn()
        self.I = I
        self.out = nc.dram_tensor("out", [NT, 128, D], F32, kind="ExternalOutput").ap()
        self.Qs = nc.dram_tensor("Qs", [8, 128, T], BF16, kind="Internal").ap()
        self.Ks = nc.dram_tensor("Ks", [8, 128, 2 * T], BF16, kind="Internal").ap()
        self.Vs = nc.dram_tensor("Vs", [8, 2 * NT, 128, 128], BF16, kind="Internal").ap()

        with ExitStack() as es:
            self.S = S = Sched(nc, es)
            self.h = self.sb(es, [128, NT, D], F32, "h")
            self.hR = [S.res("h%d" % i) for i in range(NT)]
            self.hnTR = [S.res("hnT%d" % i) for i in range(NT)]
            self.gnorm = self.sb(es, [128, D], F32, "gnorm")
            self.gnormR = S.res("gnorm")
            self.ident = self.sb(es, [128, 128], BF16, "ident")
            self.ones = self.sb(es, [128, 128], BF16, "ones")
            self.rope = self.sb(es, [128, 2, 32, 16], F32, "rope")
            self.convw = self.sb(es, [128, 12], F32, "convw")
            self.lng = self.sb(es, [128, 4], F32, "lng")
            self.WcT = self.sb(es, [128, 8, 128], BF16, "WcT")
            self.biasf = self.sb(es, [128, 4, 128], F32, "biasf")
            self.halo = self.sb(es, [128, 4, 2], F32, "halo")
            self.gq = self.sb(es, [128, 128], F32, "gq")
            self.gk = self.sb(es, [128, 128], F32, "gk")
            self.negB = self.sb(es, [128, 1], F32, "negB")
            self.dmask = self.sb(es, [128, 512], BF16, "dmask")
            self.gbias = self.sb(es, [128, 256], F32, "gbias")
            self.gates = self.sb(es, [128, NT, 8], F32, "gates")
            self.constR = S.res("consts")
            self.haloR = S.res("halo")
            self.gatesR = S.res("gates")
            self.ps = [es.enter_context(nc.psum_tensor("ps%d" % i, [128, 512], F32)) for i in range(6)]
            self.psR = [S.res("ps%d" % i, excl=True) for i in range(6)]
            self.pst = [es.enter_context(nc.psum_tensor("pst%d" % i, [128, 1024], BF16)) for i in range(2)]
            self.pstR = [S.res("pst0", excl=True), S.res("pst1", excl=True)]

            self.p12 = self.sb(es, [128, 2, NT], F32, "p12")
            self.slotu = self.sb(es, [128, 2, NT], U32, "slotu")
            self.idxw = self.sb(es, [128, NGRP, 28], U32, "idxw")
            self.idxd = self.sb(es, [128, NGRP, 14], U32, "idxd")
            self.trilb = self.sb(es, [128, 128], BF16, "trilb")
            self.giota = self.sb(es, [128, NGRP], F32, "giota")
            self.basew = self.sb(es, [128, 28], F32, "basew")
            self.based = self.sb(es, [128, 14], F32, "based")
            self.routeR = S.res("route")
            self.xgD = nc.dram_tensor("xgD", [NSLOT, D], BF16, kind="Internal").ap()
            self.yD = nc.dram_tensor("yD", [NSLOT, D], F32, kind="Internal").ap()
            es_hn = ExitStack()
            self.hnT = self.sb(es_hn, [128, 8, T], BF16, "hnT")

            self.phase_setup()
            steps = []
            for seg in (0, 1):
                steps += [("norm0", seg), ("mix", seg), ("norm1", seg), ("ffn", seg), ("norm2", seg), ("qkv", seg)]
            steps += [("attn", 1), ("norm3", 1), ("moe", 1)]
            stop = self.stop_after
            if stop != "setup":
                for (st, seg) in steps:
                    if st == "norm0":
                        self.phase_norm(0, load=("xp" if seg == 0 else "xo"))
                    elif st == "mix":
                        self.phase_mix(seg)
                    elif st == "norm1":
                        self.phase_norm(1)
                    elif st == "ffn":
                        self.phase_ffn(I["egu"], I["edn"], [L0_BLOCKS], None, post_norm=2)
                    elif st == "norm2":
                        pass
                    elif st == "qkv":
                        self.phase_qkv(seg)
                    elif st == "attn":
                        self.phase_attn()
                    elif st == "norm3":
                        self.phase_norm(3, router=True)
                    elif st == "moe":
                        if SPARSE:
                            es_hn.close()
                            self.phase_moe_sparse()
                            self.phase_combine()
                            return nc
                        self.phase_ffn(I["mgu"], I["mdn"], [MOE_BLOCKS] * NE, self.gates)
                    if stop == "%s_%d" % (st, seg):
                        break
            es_hn.close()
            self.phase_store()
        return nc

    def phase_setup(self):
        S, I = self.S, self.I
        with ExitStack() as es:
            cR = self.constR
            tmpw = self.sb(es, [128, 8, 128], F32, "tmpw")
            tril = self.sb(es, [128, 128], F32, "tril")
            bmat = self.sb(es, [128, 512], BF16, "bmat")
            bsp = self.sb(es, [128, 512], F32, "bsp")
            mq = self.sb(es, [128, 2], F32, "mq")
            rs = [S.res("su%d" % i) for i in range(16)]
            S.add("sp", i_dma(self.rope[:], I["rope"]), writes=[rs[0]], key=rs[0])
            S.add("sp", i_dma(self.convw[:], I["convw"]), writes=[rs[1]], key=rs[1])
            S.add("sp", i_dma(self.lng[:], I["lng"]), writes=[rs[2]], key=rs[2])
            S.add("sp", i_dma(self.gq[:], I["gq"]), writes=[rs[3]], key=rs[3])
            S.add("sp", i_dma(self.gk[:], I["gk"]), writes=[rs[4]], key=rs[4])
            S.add("sp", i_dma(self.gbias[:], I["gbias"]), writes=[rs[5]], key=rs[5])
            S.add("sp", i_dma(tmpw[:], I["wsT"]), writes=[rs[6]], key=rs[6])
            S.add("sp", i_dma(tril[:], I["tril"]), writes=[rs[7]], key=rs[7])
            S.add("sp", i_dma(bsp[:], I["bsp"]), writes=[rs[8]], key=rs[8])
            S.add("pool", i_dma(self.ident[:], I["ident"]), writes=[rs[9]], key=rs[9])
            S.add("pool", i_dma(self.dmask[:], I["dmask"]), writes=[rs[10]], key=rs[10])
            S.add("pool", i_dma(bmat[:], I["lnb"]), writes=[rs[12]], key=rs[12])
            S.add("dve", i_memset(self.ones[:], 1.0), writes=[rs[13]])
            if SPARSE:
                rx = [S.res("sx%d" % i) for i in range(4)]
                S.add("pool", i_dma(self.trilb[:], I["trilb"]), writes=[rx[0]], key=rx[0])
                S.add("sp", i_dma(self.giota[:], I["giota"]), writes=[rx[1]], key=rx[1])
                S.add("sp", i_dma(self.basew[:], I["basew"]), writes=[rx[2]], key=rx[2])
                S.add("sp", i_dma(self.based[:], I["based"]), writes=[rx[3]], key=rx[3])
            S.add("dve", i_memset(self.halo[:], 0.0), writes=[self.haloR])
            S.add("dve", i_red(mq[:, 0:1], self.gq[:], ALU.max, absval=True), reads=[rs[3]], writes=[rs[14]])
            S.add("dve", i_red(mq[:, 1:2], self.gk[:], ALU.max, absval=True), reads=[rs[4]], writes=[rs[15]])
            S.add("dve", i_ts(self.negB[:], mq[:, 0:1], mq[:, 1:2], -SCALE * 128.0, ALU.mult, ALU.mult),
                  reads=[rs[14], rs[15]], writes=[cR])
            S.add("dve", i_ts(self.gq[:], self.gq[:], 128.0 ** 0.5, None, ALU.mult), reads=[rs[14]], writes=[rs[3]])
            S.add("dve", i_ts(self.gk[:], self.gk[:], 128.0 ** 0.5, None, ALU.mult), reads=[rs[15]], writes=[rs[4]])
            S.add("dve", i_tt(self.WcT[:], tmpw[:], tril[:].unsqueeze(1).to_broadcast([128, 8, 128]), ALU.mult),
                  reads=[rs[6], rs[7]], writes=[rs[6]])
            for j in range(4):
                for hf in range(2):
                    g = 2 * j + hf
                    S.add("pe", i_mm(self.ps[0][hf * 64:(hf + 1) * 64, j * 128:(j + 1) * 128],
                                      bmat[:, g * 64:(g + 1) * 64], self.WcT[:, g, :], True, True),
                          reads=[rs[12], rs[6]], writes=[self.psR[0]])
            S.add("dve", i_tt(self.biasf[:].rearrange("p j t -> p (j t)"), self.ps[0][:], bsp[:], ALU.add),
                  reads=[self.psR[0], rs[8]], writes=[cR])
            S.end()

    def phase_norm(self, gi, load=None, router=False, inline_es=None):
        S, I = self.S, self.I
        with (ExitStack() if inline_es is None else _Borrow(inline_es)) as es:
            junk = [self.sb(es, [128, D], BF16, "junk") for _ in range(2)]
            junkR = [S.res("junk0"), S.res("junk1")]
            if router and SPARSE:
                hn_all = self.sb(es, [128, NT, D], BF16, "hn_all")
                hn = [hn_all[:, i, :] for i in range(NT)]
                hnR = [S.res("hna%d" % i) for i in range(NT)]
            else:
                hn2 = [self.sb(es, [128, D], BF16, "hn") for _ in range(2)]
                hn = [hn2[i % 2][:] for i in range(NT)]
                hnR2 = [S.res("hn0"), S.res("hn1")]
                hnR = [hnR2[i % 2] for i in range(NT)]
            ss = self.sb(es, [128, NT], F32, "ss")
            rsd = self.sb(es, [128, NT], F32, "rsd")
            ssR = [S.res("ss%d" % i) for i in range(NT)]
            rsR = [S.res("rs%d" % i) for i in range(NT)]
            S.add("sp", i_dma(self.gnorm[:], I["gn"][gi]), writes=[self.gnormR], key=self.gnormR)
            S.add("dve", i_ts(self.gnorm[:], self.gnorm[:], 32.0, None, ALU.mult), reads=[self.gnormR], writes=[self.gnormR])
            if load is not None:
                for i in range(NT):
                    S.add("sp", i_dma(self.h[:, i, :], I[load][i]), writes=[self.hR[i]], key=self.hR[i])
            if router:
                wr = self.sb(es, [128, 8, 8], BF16, "wr")
                wrR = S.res("wr")
                S.add("pool", i_dma(wr[:], I["wr"]), writes=[wrR], key=wrR)
            S.add("dve", i_memset(ss[:], 0.0), writes=ssR)
            for i in range(NT):
                sl = i % 2
                S.add("act", i_act(junk[sl][:], self.h[:, i, :], AF.Square, accum_out=ss[:, i:i + 1]),
                      reads=[self.hR[i]], writes=[junkR[sl], ssR[i]])
            S.add("act", i_act(rsd[:], ss[:], AF.Sqrt, bias=D * EPS, scale=1.0), reads=ssR, writes=rsR)
            S.add("dve", i_recip(rsd[:], rsd[:]), reads=rsR, writes=rsR)
            for i in range(NT):
                sl = i % 2
                S.add("dve", i_stt(hn[i], self.h[:, i, :], rsd[:, i:i + 1], self.gnorm[:], ALU.mult, ALU.mult),
                      reads=[self.hR[i], rsR[i], self.gnormR], writes=[hnR[i]])
                for c in range(8):
                    S.add("pe", i_tr(self.pst[sl][:, c * 128:(c + 1) * 128], hn[i][:, c * 128:(c + 1) * 128], self.ident[:]),
                          reads=[hnR[i]], writes=[self.pstR[sl]])
                S.add("act", i_acopy(self.hnT[:, :, i * 128:(i + 1) * 128],
                                      self.pst[sl][:].rearrange("p (c t) -> p c t", c=8)),
                      reads=[self.pstR[sl]], writes=[self.hnTR[i]])
            if router and SPARSE:
                self.router_sparse(es, wr, wrR, hn, hnR)
            elif router:
                self.router(es, wr, wrR)
            if inline_es is None:
                S.end()

    def router(self, es, wr, wrR):
        S = self.S
        lg = self.sb(es, [128, NT, 8], F32, "lg")
        mx = self.sb(es, [128, NT, 8], F32, "mx")
        tmp = self.sb(es, [128, 4, NT], F32, "rtmp")
        eq = self.sb(es, [128, 2, NT, 8], F32, "eq")
        lgR, mxR, tR, eR = S.res("lg"), S.res("mx"), S.res("rtmp"), S.res("eq")
        psr = self.ps[0]
        for i in range(NT):
            for c in range(8):
                S.add("pe", i_mm(psr[:, i * 8:(i + 1) * 8], self.hnT[:, c, i * 128:(i + 1) * 128], wr[:, c, :],
                                  c == 0, c == 7), reads=[self.hnTR[i], wrR], writes=[self.psR[0]])
        S.add("dve", i_copy(lg[:].rearrange("p t e -> p (t e)"), psr[:, 0:NT * 8]), reads=[self.psR[0]], writes=[lgR])
        for i in range(NT):
            S.add("dve", i_max8(mx[:, i, :], lg[:, i, :]), reads=[lgR], writes=[mxR])
        S.add("dve", i_tt(tmp[:, 0, :], mx[:, :, 1], mx[:, :, 0], ALU.subtract), reads=[mxR], writes=[tR])
        S.add("act", i_act(tmp[:, 1, :], tmp[:, 0, :], AF.Exp), reads=[tR], writes=[tR])
        S.add("dve", i_ts(tmp[:, 2, :], tmp[:, 1, :], 1.0, None, ALU.add), reads=[tR], writes=[tR])
        S.add("dve", i_recip(tmp[:, 2, :], tmp[:, 2, :]), reads=[tR], writes=[tR])
        S.add("dve", i_tt(tmp[:, 3, :], tmp[:, 1, :], tmp[:, 2, :], ALU.mult), reads=[tR], writes=[tR])
        for k in range(2):
            S.add("dve", i_tt(eq[:, k, :, :], lg[:], mx[:, :, k:k + 1].to_broadcast([128, NT, 8]), ALU.is_equal),
                  reads=[lgR, mxR], writes=[eR])
            S.add("dve", i_tt(eq[:, k, :, :], eq[:, k, :, :], tmp[:, 2 + k, :].unsqueeze(2).to_broadcast([128, NT, 8]), ALU.mult),
                  reads=[eR, tR], writes=[eR])
        S.add("dve", i_tt(self.gates[:], eq[:, 0, :, :], eq[:, 1, :, :], ALU.add), reads=[eR], writes=[self.gatesR])

    def phase_mix(self, seg):
        S, I = self.S, self.I
        with ExitStack() as es:
            win = self.sb(es, [128, 8, 2560], BF16, "win")
            winR = [S.res("win%d" % k) for k in range(8)]
            wout = self.sb(es, [128, 8, D], BF16, "wout")
            woutR = [S.res("wout%d" % k) for k in range(2)]
            ycat = [self.sb(es, [128, 8, 512], BF16, "ycat") for _ in range(2)]
            ycR = [[S.res("yc%d_%d" % (s, f)) for f in range(8)] for s in range(2)]
            vhat = [self.sb(es, [128, 512], BF16, "vhat") for _ in range(4)]
            vhR = [S.res("vh%d" % i) for i in range(4)]
            zin = self.sb(es, [128, 4, 514], F32, "zin")
            zR = [S.res("zin%d" % j) for j in range(4)]
            ah = self.sb(es, [128, 512], F32, "ah")
            ahR = S.res("ah")
            t1 = self.sb(es, [128, 512], F32, "t1")
            t1R = S.res("t1")
            tmpb = self.sb(es, [128, 512], F32, "tmpb")
            tbR = S.res("tmpb")
            st = self.sb(es, [128, 4, 6], F32, "bnst")
            mv = self.sb(es, [128, 4, 4], F32, "bnmv")
            stR = [S.res("st%d" % i) for i in range(4)]
            for k in range(8):
                S.add("pool", i_dma(win[:, k, :], I["win"][:, k, :]), writes=[winR[k]], key=winR[k])
            for k in range(2):
                S.add("pool", i_dma(wout[:, 4 * k:4 * k + 4, :], I["wout"][:, 4 * k:4 * k + 4, :]), writes=[woutR[k]], key=woutR[k])
            S.add("dve", i_copy(zin[:, :, 0:2], self.halo[:]), reads=[self.haloR], writes=zR)
            cR = self.constR
            ps, psR = self.ps, self.psR
            def make_group(g):
                ys, yR = ycat[g % 2], ycR[g % 2]
                gcols = slice(g * 512, (g + 1) * 512)
                gt = [self.hnTR[4 * g + ti] for ti in range(4)]

                def fA():
                    for ti in range(4):
                        tile = 4 * g + ti
                        b = ti % 2
                        for c in range(8):
                            S.add("pe", i_mm(ps[b][:], self.hnT[:, c, tile * 128:(tile + 1) * 128], win[:, c, 2048:2560],
                                              c == 0, c == 7), reads=[self.hnTR[tile], winR[c]], writes=[psR[b]])
                        S.add("dve", i_bnstats(st[:, ti, :], ps[b][:]), reads=[psR[b]], writes=[stR[ti]])
                        S.add("dve", i_bnaggr(mv[:, ti, 0:2], st[:, ti, :]), reads=[stR[ti]], writes=[stR[ti]])
                        S.add("act", i_act(mv[:, ti, 2:3], mv[:, ti, 1:2], AF.Sqrt, bias=EPS, scale=1.0), reads=[stR[ti]], writes=[stR[ti]])
                        S.add("dve", i_recip(mv[:, ti, 2:3], mv[:, ti, 2:3]), reads=[stR[ti]], writes=[stR[ti]])
                        S.add("dve", i_ts(mv[:, ti, 3:4], mv[:, ti, 0:1], mv[:, ti, 2:3], -1.0, ALU.mult, ALU.mult),
                              reads=[stR[ti]], writes=[stR[ti]])
                        S.add("act", i_act(vhat[ti][:], ps[b][:], AF.Identity, bias=mv[:, ti, 3:4], scale=mv[:, ti, 2:3]),
                              reads=[psR[b], stR[ti]], writes=[vhR[ti]])

                def fBC():
                    for j in range(4):
                        for ti in range(4):
                            for hf in range(2):
                                gg = 2 * j + hf
                                S.add("pe", i_mm(ps[2][hf * 64:(hf + 1) * 64, ti * 128:(ti + 1) * 128],
                                                  vhat[ti][:, gg * 64:(gg + 1) * 64], self.WcT[:, gg, :], True, True),
                                      reads=[vhR[ti], cR], writes=[psR[2]])
                        for c in range(8):
                            S.add("pe", i_mm(ps[3][:], win[:, c, 1536 + j * 128:1536 + (j + 1) * 128], self.hnT[:, c, gcols],
                                              c == 0, c == 7), reads=gt + [winR[c]], writes=[psR[3]])
                        S.add("dve", i_stt(tmpb[:].rearrange("p (a t) -> p a t", a=4), ps[2][:].rearrange("p (a t) -> p a t", a=4),
                                            self.lng[:, j:j + 1],
                                            self.biasf[:, j, :].unsqueeze(1).to_broadcast([128, 4, 128]), ALU.mult, ALU.add),
                              reads=[psR[2], cR], writes=[tbR])
                        S.add("dve", i_tt(ys[:, 4 + j, :], tmpb[:], ps[3][:], ALU.mult), reads=[tbR, psR[3]], writes=[yR[4 + j]])

                def fD():
                    for j in range(4):
                        for part in range(3):
                            for c in range(8):
                                S.add("pe", i_mm(ps[4 + part % 2][:], win[:, c, part * 512 + j * 128: part * 512 + (j + 1) * 128],
                                                  self.hnT[:, c, gcols], c == 0, c == 7),
                                      reads=gt + [winR[c]], writes=[psR[4 + part % 2]])
                            if part == 0:
                                S.add("act", i_acopy(ah[:], ps[4][:]), reads=[psR[4]], writes=[ahR])
                            elif part == 1:
                                S.add("dve", i_tt(zin[:, j, 2:514], ps[5][:], ah[:], ALU.mult), reads=[psR[5], ahR], writes=[zR[j]])
                        S.add("dve", i_ts(t1[:], zin[:, j, 2:514], self.convw[:, 3 * j + 2:3 * j + 3], None, ALU.mult),
                              reads=[zR[j], cR], writes=[t1R])
                        S.add("dve", i_stt(t1[:], zin[:, j, 1:513], self.convw[:, 3 * j + 1:3 * j + 2], t1[:], ALU.mult, ALU.add),
                              reads=[zR[j], t1R], writes=[t1R])
                        S.add("dve", i_stt(t1[:], zin[:, j, 0:512], self.convw[:, 3 * j:3 * j + 1], t1[:], ALU.mult, ALU.add),
                              reads=[zR[j], t1R], writes=[t1R])
                        S.add("dve", i_tt(ys[:, j, :], ps[4][:], t1[:], ALU.mult), reads=[psR[4], t1R], writes=[yR[j]])
                        S.add("dve", i_copy(zin[:, j, 0:2], zin[:, j, 512:514]), reads=[zR[j]], writes=[zR[j]])

                def fE():
                    for ti in range(4):
                        tile = 4 * g + ti
                        for hf in range(2):
                            b = hf
                            for f in range(8):
                                S.add("pe", i_mm(ps[b][:], ys[:, f, ti * 128:(ti + 1) * 128], wout[:, f, hf * 512:(hf + 1) * 512],
                                                  f == 0, f == 7), reads=[yR[f], woutR[f // 4]], writes=[psR[b]])
                            S.add("dve", i_tt(self.h[:, tile, hf * 512:(hf + 1) * 512], ps[b][:], self.h[:, tile, hf * 512:(hf + 1) * 512], ALU.add),
                                  reads=[psR[b], self.hR[tile]], writes=[self.hR[tile]])
                return fA, fBC, fD, fE

            prevE = None
            for g in range(NG):
                fA, fBC, fD, fE = make_group(g)
                fA()
                fD()
                if prevE is not None:
                    prevE()
                fBC()
                prevE = fE
            prevE()
            S.add("dve", i_copy(self.halo[:], zin[:, :, 512:514]), reads=zR, writes=[self.haloR])
            S.end()

    def phase_ffn(self, gu_d, dn_d, expert_blocks, gates, post_norm=None):
        S = self.S
        ne = len(expert_blocks)
        with ExitStack() as es:
            wgu = [self.sb(es, [128, 8, 2, 512], BF16, "wgu") for _ in range(2)]
            wdn = [self.sb(es, [128, 4, D], BF16, "wdn") for _ in range(2)]
            wguR = [S.res("wgu0"), S.res("wgu1")]
            wdnR = [S.res("wdn0"), S.res("wdn1")]
            Y = [self.sb(es, [128, 4, 512], BF16, "Y") for _ in range(2)]
            YR = [[S.res("Y%d_%d" % (s, f)) for f in range(4)] for s in range(2)]
            sg = [self.sb(es, [128, 512], F32, "sg") for _ in range(2)]
            sgR = [S.res("sg0"), S.res("sg1")]
            ps, psR = self.ps, self.psR
            blocks = []
            for e in range(ne):
                f0 = 0
                for w in expert_blocks[e]:
                    blocks.append((e, f0, w))
                    f0 += w
            def issue(bi):
                e, f0, w = blocks[bi]
                sl = bi % 2
                nch = w // 128
                if ne == 1:
                    src_gu = gu_d[:, 16 * f0:16 * (f0 + w)]
                    src_dn = dn_d[:, (f0 // 128) * D:(f0 // 128 + nch) * D]
                else:
                    src_gu = gu_d[e, :, 16 * f0:16 * (f0 + w)]
                    src_dn = dn_d[e, :, (f0 // 128) * D:(f0 // 128 + nch) * D]
                S.add("pool", i_dma(wgu[sl][:, :, :, 0:w], src_gu.rearrange("p (k g f) -> p k g f", k=8, g=2)),
                      writes=[wguR[sl]], key=wguR[sl])
                S.add("pool", i_dma(wdn[sl][:, 0:nch, :], src_dn.rearrange("p (c n) -> p c n", c=nch)),
                      writes=[wdnR[sl]], key=wdnR[sl])
            issue(0)
            cnt = 0
            dcnt = 0
            for bi, (e, f0, w) in enumerate(blocks):
                if bi + 1 < len(blocks):
                    issue(bi + 1)
                sl = bi % 2
                nch = w // 128
                for g in range(NG):
                    gcols = slice(g * 512, (g + 1) * 512)
                    gt = [self.hnTR[4 * g + ti] for ti in range(4)]
                    ysl = (bi * NG + g) % 2
                    for fc in range(nch):
                        pb = cnt % 2
                        cnt += 1
                        for c in range(8):
                            S.add("pe", i_mm(ps[pb][:], wgu[sl][:, c, 0, fc * 128:(fc + 1) * 128], self.hnT[:, c, gcols],
                                              c == 0, c == 7), reads=gt + [wguR[sl]], writes=[psR[pb]])
                        for c in range(8):
                            S.add("pe", i_mm(ps[2 + pb][:], wgu[sl][:, c, 1, fc * 128:(fc + 1) * 128], self.hnT[:, c, gcols],
                                              c == 0, c == 7), reads=gt + [wguR[sl]], writes=[psR[2 + pb]])
                        S.add("act", i_act(sg[pb][:], ps[pb][:], AF.Silu), reads=[psR[pb]], writes=[sgR[pb]])
                        S.add("dve", i_tt(Y[ysl][:, fc, :], sg[pb][:], ps[2 + pb][:], ALU.mult),
                              reads=[sgR[pb], psR[2 + pb]], writes=[YR[ysl][fc]])
                    for ti in range(4):
                        tile = 4 * g + ti
                        for hf in range(2):
                            db = 4 + dcnt % 2
                            dcnt += 1
                            for fc in range(nch):
                                S.add("pe", i_mm(ps[db][:], Y[ysl][:, fc, ti * 128:(ti + 1) * 128],
                                                  wdn[sl][:, fc, hf * 512:(hf + 1) * 512], fc == 0, fc == nch - 1),
                                      reads=[YR[ysl][fc], wdnR[sl]], writes=[psR[db]])
                            hsl = self.h[:, tile, hf * 512:(hf + 1) * 512]
                            if gates is None:
                                S.add("dve", i_tt(hsl, ps[db][:], hsl, ALU.add),
                                      reads=[psR[db], self.hR[tile]], writes=[self.hR[tile]])
                            else:
                                S.add("dve", i_stt(hsl, ps[db][:], gates[:, tile, e:e + 1], hsl, ALU.mult, ALU.add),
                                      reads=[psR[db], self.hR[tile], self.gatesR], writes=[self.hR[tile]])
            if post_norm is not None:
                self.phase_norm(post_norm, inline_es=es)
            S.end()

    def phase_qkv(self, seg):
        S, I = self.S, self.I
        with ExitStack() as es:
            NS = 3
            wq = [self.sb(es, [128, 8, 512], BF16, "wq") for _ in range(2)]
            wqR = [S.res("wq0"), S.res("wq1")]
            sq = [self.sb(es, [128, 128], BF16, "sq") for _ in range(2)]
            sqR = [S.res("sq0"), S.res("sq1")]
            ssq = [self.sb(es, [128, 2, 4], F32, "ssq") for _ in range(NS)]
            ssR = [S.res("ssq%d" % i) for i in range(NS)]
            qn = [self.sb(es, [128, 4, 128], F32, "qn") for _ in range(NS)]
            qnR = [S.res("qn%d" % i) for i in range(NS)]
            rt = [self.sb(es, [128, 4, 4, 16], F32, "rt") for _ in range(NS)]
            rtR = [S.res("rt%d" % i) for i in range(NS)]
            NQ = 4
            qb = [self.sb(es, [128, 4, 128], BF16, "qb") for _ in range(NQ)]
            qbR = [S.res("qb%d" % i) for i in range(NQ)]
            stg = [self.sb(es, [128, 4, 512], BF16, "stg") for _ in range(2)]
            stgR = [S.res("stg0"), S.res("stg1")]
            vst = [self.sb(es, [128, 4, 128], BF16, "vst") for _ in range(2)]
            vstR = [S.res("vst0"), S.res("vst1")]
            ps, psR = self.ps, self.psR
            cR = self.constR
            cbs = ([0, 1] if seg == 1 else []) + [2, 3, 4, 5]

            def issue(ci):
                cb = cbs[ci]
                S.add("pool", i_dma(wq[ci % 2][:], I["wqkv"][:, :, cb * 512:(cb + 1) * 512]), writes=[wqR[ci % 2]], key=wqR[ci % 2])
            issue(0)
            cnt = 0
            state = {"gcount": 0, "tcount": 0}
            pending = []

            def make_post(kind, hd0, i, qs):
                def post():
                    tb = state["tcount"] % 2
                    state["tcount"] += 1
                    for hh in range(4):
                        S.add("pe", i_tr(self.pst[tb][:, hh * 128:(hh + 1) * 128], qb[qs][:, hh, :], self.ident[:]),
                              reads=[qbR[qs], cR], writes=[self.pstR[tb]])
                    ss_ = state["gcount"] % 2
                    ti = i % 4
                    S.add("act", i_acopy(stg[ss_][:, :, ti * 128:(ti + 1) * 128],
                                          self.pst[tb][:, 0:512].rearrange("p (h t) -> p h t", h=4)),
                          reads=[self.pstR[tb]], writes=[stgR[ss_]])
                    if ti == 3:
                        g = i // 4
                        if kind == 0:
                            dst = self.Qs[hd0:hd0 + 4, :, g * 512:(g + 1) * 512]
                        else:
                            dst = self.Ks[hd0:hd0 + 4, :, seg * T + g * 512: seg * T + (g + 1) * 512]
                        S.add("sp", i_dma(dst.rearrange("h d t -> d h t"), stg[ss_][:]), reads=[stgR[ss_]], key=stgR[ss_])
                        state["gcount"] += 1
                return post

            pendB = []

            def make_B(kind, pb, s_, qs, gtile):
                gvec = self.gq if kind == 0 else self.gk
                r_ = rt[s_]
                x1 = qn[s_][:, :, 0:16]
                x2 = qn[s_][:, :, 16:32]
                cosb = self.rope[:, 0, gtile, :].unsqueeze(1).to_broadcast([128, 4, 16])
                sinb = self.rope[:, 1, gtile, :].unsqueeze(1).to_broadcast([128, 4, 16])

                def part1():
                    for hh in range(4):
                        S.add("dve", i_stt(qn[s_][:, hh, :], ps[pb][:, hh * 128:(hh + 1) * 128], ssq[s_][:, 1, hh:hh + 1], gvec[:], ALU.mult, ALU.mult),
                              reads=[psR[pb], ssR[s_], cR], writes=[qnR[s_]])
                    S.add("act", i_acopy(qb[qs][:], qn[s_][:]), reads=[qnR[s_]], writes=[qbR[qs]])
                    S.add("dve", i_tt(r_[:, 0, :, :], x1, cosb, ALU.mult), reads=[qnR[s_], cR], writes=[rtR[s_]])
                    S.add("dve", i_tt(r_[:, 1, :, :], x2, sinb, ALU.mult), reads=[qnR[s_], cR], writes=[rtR[s_]])
                    S.add("dve", i_tt(r_[:, 2, :, :], x2, cosb, ALU.mult), reads=[qnR[s_], cR], writes=[rtR[s_]])
                    S.add("dve", i_tt(r_[:, 3, :, :], x1, sinb, ALU.mult), reads=[qnR[s_], cR], writes=[rtR[s_]])

                def part2():
                    S.add("dve", i_tt(qb[qs][:, :, 0:16], r_[:, 0, :, :], r_[:, 1, :, :], ALU.subtract), reads=[rtR[s_], qbR[qs]], writes=[qbR[qs]])
                    S.add("dve", i_tt(qb[qs][:, :, 16:32], r_[:, 2, :, :], r_[:, 3, :, :], ALU.add), reads=[rtR[s_], qbR[qs]], writes=[qbR[qs]])
                return part1, part2

            def step(tile_A):
                recip = None
                newB = None
                if tile_A is not None:
                    recip, newB = tile_A()
                if pendB:
                    p1, p2 = pendB.pop(0)
                    if p1 is not None:
                        p1()
                else:
                    p2 = None
                if recip is not None:
                    recip()
                if p2 is not None:
                    p2()
                if len(pending) >= 3 or (tile_A is None and pending):
                    pending.pop(0)()
                if tile_A is not None:
                    pendB.append(newB if newB is not None else (None, None))

            for ci, cb in enumerate(cbs):
                if ci + 1 < len(cbs):
                    issue(ci + 1)
                wsl = ci % 2
                kind = cb // 2
                hd0 = (cb % 2) * 4
                for i in range(NT):
                    def tile_A(ci=ci, cb=cb, wsl=wsl, kind=kind, hd0=hd0, i=i):
                        nonlocal cnt
                        gtile = seg * NT + i
                        pb = cnt % 3
                        s_ = cnt % NS
                        qs = cnt % NQ
                        cnt += 1
                        for c in range(8):
                            S.add("pe", i_mm(ps[pb][:], self.hnT[:, c, i * 128:(i + 1) * 128], wq[wsl][:, c, :], c == 0, c == 7),
                                  reads=[self.hnTR[i], wqR[wsl]], writes=[psR[pb]])
                        if kind == 2:
                            vs = cnt % 2
                            S.add("act", i_acopy(vst[vs][:].rearrange("p h d -> p (h d)"), ps[pb][:]), reads=[psR[pb]], writes=[vstR[vs]])
                            S.add("sp", i_dma(self.Vs[hd0:hd0 + 4, gtile, :, :].rearrange("h p d -> p h d"), vst[vs][:]),
                                  reads=[vstR[vs]], key=vstR[vs])
                            return None, None
                        S.add("dve", i_memset(ssq[s_][:, 0, :], 0.0), writes=[ssR[s_]])
                        for hh in range(4):
                            S.add("act", i_act(sq[hh % 2][:], ps[pb][:, hh * 128:(hh + 1) * 128], AF.Square, accum_out=ssq[s_][:, 0, hh:hh + 1]),
                                  reads=[psR[pb], ssR[s_]], writes=[sqR[hh % 2], ssR[s_]])
                        S.add("act", i_act(ssq[s_][:, 1, :], ssq[s_][:, 0, :], AF.Sqrt, bias=128.0 * EPS, scale=1.0), reads=[ssR[s_]], writes=[ssR[s_]])

                        def recip():
                            S.add("dve", i_recip(ssq[s_][:, 1, :], ssq[s_][:, 1, :]), reads=[ssR[s_]], writes=[ssR[s_]])
                        pending.append(make_post(kind, hd0, i, qs))
                        return recip, make_B(kind, pb, s_, qs, gtile)
                    step(tile_A)
            while pendB or pending:
                step(None)
            S.end()

    def phase_attn(self):
        S, I = self.S, self.I
        with ExitStack() as es:
            KT = [self.sb(es, [128, 2 * T], BF16, "KT") for _ in range(2)]
            QT = [self.sb(es, [128, T], BF16, "QT") for _ in range(2)]
            V = [self.sb(es, [128, 2 * NT, 128], BF16, "V") for _ in range(2)]
            KTR = [S.res("KT0"), S.res("KT1")]
            QTR = [S.res("QT0"), S.res("QT1")]
            VR = [S.res("V0"), S.res("V1")]
            wo = self.sb(es, [128, 8, D], BF16, "wo")
            woR = [S.res("wo0"), S.res("wo1")]
            NPT = 4
            PT = [self.sb(es, [128, 512], BF16, "PT") for _ in range(NPT)]
            PTR = [S.res("PT%d" % i) for i in range(NPT)]
            maskT = [self.sb(es, [128, T], BF16, "maskT") for _ in range(2)]
            mTR = [S.res("maskT0"), S.res("maskT1")]
            km = self.sb(es, [128, 16], F32, "km")
            kmb = self.sb(es, [128, 16], BF16, "kmb")
            kmR = S.res("km")
            gm = self.sb(es, [128, 16, 16], F32, "gm")
            gmR = S.res("gm")
            mx = self.sb(es, [128, 16, 8], F32, "amx")
            mxR = S.res("amx")
            thr = self.sb(es, [128, 16], F32, "thr")
            thR = S.res("thr")
            sel = self.sb(es, [128, 16, 16], F32, "sel")
            selR = S.res("sel")
            mb = self.sb(es, [128, 256], BF16, "mb")
            mbR = S.res("mb")
            rsum = self.sb(es, [128, 256], F32, "rsum")
            rsR = S.res("rsum")
            ps, psR = self.ps, self.psR
            cR = self.constR
            oTR = self.hnTR

            for k in range(2):
                S.add("pool", i_dma(wo[:, 4 * k:4 * k + 4, :], I["wo"][:, 4 * k:4 * k + 4, :]), writes=[woR[k]], key=woR[k])
            self.eoh = self.sb(es, [128, 2048], BF16, "eoh")
            eohR = S.res("eoh")
            S.add("pool", i_dma(self.eoh[:], I["eoh"]), writes=[eohR], key=eohR)
            for k in range(2):
                S.add("dve", i_memset(maskT[k][:], 0.0), writes=[mTR[k]])

            def load(hd):
                sl = hd % 2
                S.add("sp", i_dma(KT[sl][:], self.Ks[hd]), writes=[KTR[sl]], key=KTR[sl])
                S.add("sp", i_dma(QT[sl][:], self.Qs[hd]), writes=[QTR[sl]], key=QTR[sl])
                S.add("sp", i_dma(V[sl][:], self.Vs[hd].rearrange("n p d -> p n d")), writes=[VR[sl]], key=VR[sl])

            def pro1(hd):
                sl = hd % 2
                S.add("dve", i_red(km[:], KT[sl][:].rearrange("p (n k) -> p n k", n=16), ALU.add), reads=[KTR[sl]], writes=[kmR])
                S.add("dve", i_ts(kmb[:], km[:], 1.0 / 256.0, None, ALU.mult), reads=[kmR], writes=[kmR])

            def pro2(hd):
                sl = hd % 2
                for i in range(NT):
                    S.add("pe", i_mm(ps[5][:, i * 16:(i + 1) * 16], QT[sl][:, i * 128:(i + 1) * 128], kmb[:], True, True),
                          reads=[QTR[sl], kmR], writes=[psR[5]])
                S.add("dve", i_tt(gm[:].rearrange("p a b -> p (a b)"), ps[5][:, 0:256], self.gbias[:], ALU.add),
                      reads=[psR[5], cR], writes=[gmR])
                for i in range(NT):
                    S.add("dve", i_max8(mx[:, i, :], gm[:, i, :]), reads=[gmR], writes=[mxR])
                S.add("dve", i_ts(thr[:], mx[:, :, 2], -1e29, None, ALU.max), reads=[mxR], writes=[thR])
                S.add("dve", i_tt(sel[:], gm[:], thr[:].unsqueeze(2).to_broadcast([128, 16, 16]), ALU.is_ge),
                      reads=[gmR, thR], writes=[selR])
                S.add("dve", i_ts(mb[:], sel[:].rearrange("p a b -> p (a b)"), -1.0, 1e5, ALU.add, ALU.mult), reads=[selR], writes=[mbR])

            def pro3(hd):
                sl = hd % 2
                for half in range(2):
                    for i8 in range(8):
                        i = half * 8 + i8
                        S.add("pe", i_tr(self.pst[half][0:16, i8 * 128:(i8 + 1) * 128], mb[:, i * 16:(i + 1) * 16], self.ident[:]),
                              reads=[mbR, cR], writes=[self.pstR[half]])
                    S.add("act", i_acopy(maskT[sl][0:16, half * 1024:(half + 1) * 1024], self.pst[half][0:16, :]),
                          reads=[self.pstR[half]], writes=[mTR[sl]])

            iters = [(hd, j, n) for hd in range(8) for j in range(8) for n in range(9 + j)]
            state = {}

            def partA(k):
                hd, j, n = iters[k]
                sl = hd % 2
                qcols = slice(j * 256, (j + 1) * 256)
                diag = (n == 8 + j)
                sb_ = k % 3
                for a in range(2):
                    S.add("pe", i_mm(ps[sb_][:, a * 256:(a + 1) * 256], KT[sl][:, n * 256 + a * 128: n * 256 + (a + 1) * 128],
                                      QT[sl][:, qcols], True, diag), reads=[KTR[sl], QTR[sl]], writes=[psR[sb_]])
                    if not diag:
                        S.add("pe", i_mm(ps[sb_][:, a * 256:(a + 1) * 256], self.eoh[:, n * 128:(n + 1) * 128],
                                          maskT[sl][:, qcols], False, True), reads=[eohR, mTR[sl]], writes=[psR[sb_]])

            def partB(k):
                hd, j, n = iters[k]
                sl = hd % 2
                qcols = slice(j * 256, (j + 1) * 256)
                diag = (n == 8 + j)
                sb_ = k % 3
                pt = k % NPT
                ob = 3 + (j % 2)
                S.add("act", i_act(PT[pt][:], ps[sb_][:], AF.Exp, bias=self.negB[:], scale=SCALE),
                      reads=[psR[sb_], cR], writes=[PTR[pt]])
                if diag:
                    S.add("dve", i_tt(PT[pt][:], PT[pt][:], self.dmask[:], ALU.mult), reads=[PTR[pt], cR], writes=[PTR[pt]])
                state.setdefault("pv", []).append((k, pt))

            def partC(k):
                hd, j, n = iters[k]
                sl = hd % 2
                qcols = slice(j * 256, (j + 1) * 256)
                diag = (n == 8 + j)
                pt = k % NPT
                ob = 3 + (j % 2)
                for a in range(2):
                    first = (n == 0 and a == 0)
                    last = (diag and a == 1)
                    S.add("pe", i_mm(ps[ob][:, 0:256], V[sl][:, 2 * n + a, :], PT[pt][:, a * 256:(a + 1) * 256], first, last, True),
                          reads=[VR[sl], PTR[pt]], writes=[psR[ob]])
                    S.add("pe", i_mm(ps[ob][:, 256:512], self.ones[:], PT[pt][:, a * 256:(a + 1) * 256], False, last, True),
                          reads=[cR, PTR[pt]], writes=[psR[ob]])
                if diag:
                    S.add("dve", i_recip(rsum[:], ps[ob][:, 256:512]), reads=[psR[ob]], writes=[rsR])
                    S.add("dve", i_tt(self.hnT[:, hd, qcols], ps[ob][:, 0:256], rsum[:], ALU.mult),
                          reads=[psR[ob], rsR], writes=[oTR[2 * j], oTR[2 * j + 1]])

            load(0)
            pro1(0)
            pro2(0)
            pro3(0)
            load(1)
            nit = len(iters)
            partA(0)
            for k in range(nit):
                hd, j, n = iters[k]
                partB(k)
                if k + 1 < nit:
                    hd2, j2, n2 = iters[k + 1]
                    if hd2 != hd and hd2 + 1 < 8:
                        pass
                    partA(k + 1)
                partC(k)
                if hd + 1 < 8 and n == 0:
                    if j == 1:
                        pro1(hd + 1)
                    elif j == 3:
                        pro2(hd + 1)
                    elif j == 5:
                        pro3(hd + 1)
                    elif j == 7 and hd + 2 < 8:
                        pass
                if n == 8 + j and j == 7 and hd + 2 < 8:
                    load(hd + 2)
            cnt = 0
            for i in range(NT):
                for hf in range(2):
                    b = cnt % 3
                    cnt += 1
                    for f in range(8):
                        S.add("pe", i_mm(ps[b][:], self.hnT[:, f, i * 128:(i + 1) * 128], wo[:, f, hf * 512:(hf + 1) * 512], f == 0, f == 7),
                              reads=[oTR[i], woR[f // 4]], writes=[psR[b]])
                    hsl = self.h[:, i, hf * 512:(hf + 1) * 512]
                    S.add("dve", i_tt(hsl, ps[b][:], hsl, ALU.add), reads=[psR[b], self.hR[i]], writes=[self.hR[i]])
            S.end()

    def router_sparse(self, es, wr, wrR, hn, hnR):
        S = self.S
        ps, psR = self.ps, self.psR
        cR = self.constR
        lg = self.sb(es, [128, NT, 8], F32, "lg")
        mx = self.sb(es, [128, NT, 8], F32, "mx")
        tmp = self.sb(es, [128, 3, NT], F32, "rtmp")
        eq = self.sb(es, [128, 2, NT, 8], F32, "eq")
        selb = self.sb(es, [128, NT, 8], BF16, "selb")
        cum = self.sb(es, [128, NT, 8], F32, "cum")
        tot = self.sb(es, [128, 8], F32, "tot")
        ng = self.sb(es, [128, 2, 8], F32, "ng")
        off = self.sb(es, [128, 2, 8], F32, "off")
        slotv = self.sb(es, [128, NT, 8], F32, "slotv")
        t3 = self.sb(es, [128, NT, 8], F32, "t3")
        slotf = self.sb(es, [128, 2, NT], F32, "slotf")
        cmp_ = self.sb(es, [128, NGRP, 8], F32, "cmp")
        gef = self.sb(es, [128, NGRP], F32, "gef")
        iwf = self.sb(es, [128, NGRP, 28], F32, "iwf")
        idf = self.sb(es, [128, NGRP, 14], F32, "idf")
        lgR, mxR, tR, eR, sbR, cuR, toR, ngR, ofR, svR, t3R, sfR, cmR, geR, iwR, idR = [S.res("rs_%d" % i) for i in range(16)]
        rR = self.routeR
        psr = ps[0]
        for i in range(NT):
            for c in range(8):
                S.add("pe", i_mm(psr[:, i * 8:(i + 1) * 8], self.hnT[:, c, i * 128:(i + 1) * 128], wr[:, c, :],
                                  c == 0, c == 7), reads=[self.hnTR[i], wrR], writes=[psR[0]])
        S.add("dve", i_copy(lg[:].rearrange("p t e -> p (t e)"), psr[:, 0:NT * 8]), reads=[psR[0]], writes=[lgR])
        for i in range(NT):
            S.add("dve", i_max8(mx[:, i, :], lg[:, i, :]), reads=[lgR], writes=[mxR])
        S.add("dve", i_tt(tmp[:, 0, :], mx[:, :, 1], mx[:, :, 0], ALU.subtract), reads=[mxR], writes=[tR])
        S.add("act", i_act(tmp[:, 1, :], tmp[:, 0, :], AF.Exp), reads=[tR], writes=[tR])
        S.add("dve", i_ts(tmp[:, 2, :], tmp[:, 1, :], 1.0, None, ALU.add), reads=[tR], writes=[tR])
        S.add("dve", i_recip(self.p12[:, 0, :], tmp[:, 2, :]), reads=[tR], writes=[rR])
        S.add("dve", i_tt(self.p12[:, 1, :], tmp[:, 1, :], self.p12[:, 0, :], ALU.mult), reads=[tR, rR], writes=[rR])
        for k in range(2):
            S.add("dve", i_tt(eq[:, k, :, :], lg[:], mx[:, :, k:k + 1].to_broadcast([128, NT, 8]), ALU.is_equal),
                  reads=[lgR, mxR], writes=[eR])
        S.add("dve", i_tt(selb[:], eq[:, 0, :, :], eq[:, 1, :, :], ALU.add), reads=[eR], writes=[sbR])
        for i in range(NT):
            for i2 in range(i):
                S.add("pe", i_mm(ps[1][:, i * 8:(i + 1) * 8], self.ones[:], selb[:, i2, :], i2 == 0, False),
                      reads=[sbR, cR], writes=[psR[1]])
            S.add("pe", i_mm(ps[1][:, i * 8:(i + 1) * 8], self.trilb[:], selb[:, i, :], i == 0, True),
                  reads=[sbR, cR], writes=[psR[1]])
        for i2 in range(NT):
            S.add("pe", i_mm(ps[2][:, 0:8], self.ones[:], selb[:, i2, :], i2 == 0, i2 == NT - 1),
                  reads=[sbR, cR], writes=[psR[2]])
        S.add("dve", i_copy(cum[:].rearrange("p t e -> p (t e)"), ps[1][:, 0:NT * 8]), reads=[psR[1]], writes=[cuR])
        S.add("dve", i_copy(tot[:], ps[2][:, 0:8]), reads=[psR[2]], writes=[toR])
        S.add("dve", i_ts(ng[:, 0, :], tot[:], 0.5, None, ALU.is_gt), reads=[toR], writes=[ngR])
        for thr_ in (512.5, 1024.5, 1536.5):
            S.add("dve", i_ts(ng[:, 1, :], tot[:], thr_, None, ALU.is_gt), reads=[toR, ngR], writes=[ngR])
            S.add("dve", i_tt(ng[:, 0, :], ng[:, 0, :], ng[:, 1, :], ALU.add), reads=[ngR], writes=[ngR])
        S.add("dve", i_memset(off[:], 0.0), writes=[ofR])
        for e in range(1, 8):
            S.add("dve", i_tt(off[:, 0, e:e + 1], off[:, 0, e - 1:e], ng[:, 0, e - 1:e], ALU.add), reads=[ofR, ngR], writes=[ofR])
        S.add("dve", i_ts(off[:, 1, :], off[:, 0, :], 512.0, -1.0, ALU.mult, ALU.add), reads=[ofR], writes=[ofR])
        S.add("dve", i_tt(slotv[:], cum[:], off[:, 1, :].unsqueeze(1).to_broadcast([128, NT, 8]), ALU.add),
              reads=[cuR, ofR], writes=[svR])
        for k in range(2):
            S.add("dve", i_tt(t3[:], eq[:, k, :, :], slotv[:], ALU.mult), reads=[eR, svR, t3R], writes=[t3R])
            S.add("dve", i_red(slotf[:, k, :], t3[:], ALU.add), reads=[t3R], writes=[sfR])
        S.add("dve", i_copy(self.slotu[:], slotf[:]), reads=[sfR], writes=[rR])
        S.add("dve", i_tt(cmp_[:], off[:, 0, :].unsqueeze(1).to_broadcast([128, NGRP, 8]),
                          self.giota[:].unsqueeze(2).to_broadcast([128, NGRP, 8]), ALU.is_le), reads=[ofR, cR], writes=[cmR])
        S.add("dve", i_red(gef[:], cmp_[:], ALU.add), reads=[cmR], writes=[geR])
        S.add("dve", i_ts(gef[:], gef[:], -1.0, None, ALU.add), reads=[geR], writes=[geR])
        S.add("dve", i_stt(iwf[:], gef[:].unsqueeze(2).to_broadcast([128, NGRP, 28]), 3584.0,
                           self.basew[:].unsqueeze(1).to_broadcast([128, NGRP, 28]), ALU.mult, ALU.add),
              reads=[geR, cR], writes=[iwR])
        S.add("dve", i_stt(idf[:], gef[:].unsqueeze(2).to_broadcast([128, NGRP, 14]), 1792.0,
                           self.based[:].unsqueeze(1).to_broadcast([128, NGRP, 14]), ALU.mult, ALU.add),
              reads=[geR, cR], writes=[idR])
        S.add("dve", i_copy(self.idxw[:], iwf[:]), reads=[iwR], writes=[rR])
        S.add("dve", i_copy(self.idxd[:], idf[:]), reads=[idR], writes=[rR])
        for i in range(NT):
            for k in range(2):
                S.add("pool", i_scatter(self.xgD, self.slotu[:, k, i:i + 1], hn[i], NSLOT - 1),
                      reads=[hnR[i], rR], key=hnR[i])

    def phase_moe_sparse(self):
        S, I = self.S, self.I
        mgu2, mdn2 = I["mgu"], I["mdn"]
        with ExitStack() as es:
            NW = 3
            wgu = [self.sb(es, [128, 8, 2, 512], BF16, "wgu") for _ in range(NW)]
            wdn = [self.sb(es, [128, 4, D], BF16, "wdn") for _ in range(NW)]
            wguR = [[S.res("wgu%d_%d" % (s_, q)) for q in range(4)] for s_ in range(NW)]
            wdnR = [[S.res("wdn%d_%d" % (s_, q)) for q in range(2)] for s_ in range(NW)]
            Y = [self.sb(es, [128, 4, 512], BF16, "Y") for _ in range(2)]
            YR = [[S.res("Y%d_%d" % (s_, f)) for f in range(4)] for s_ in range(2)]
            sg = [self.sb(es, [128, 512], F32, "sg") for _ in range(2)]
            sgR = [S.res("sg0"), S.res("sg1")]
            xtm = [self.sb(es, [128, D], BF16, "xtm") for _ in range(2)]
            xtmR = [S.res("xtm0"), S.res("xtm1")]
            xgT = [self.sb(es, [128, 8, 512], BF16, "xgT") for _ in range(2)]
            xgTR = [[S.res("xgT%d_%d" % (s_, t)) for t in range(4)] for s_ in range(2)]
            yacc = self.sb(es, [128, 4, D], F32, "yacc")
            yaccR = [S.res("yacc%d" % t) for t in range(4)]
            ps, psR = self.ps, self.psR
            rR = self.routeR

            def issue_w(g, fb):
                sl = (g * 7 + fb) % NW
                wf = wgu[sl][:].rearrange("p k g f -> p (k g f)")
                for q in range(4):
                    S.add("pool", i_gather(wf[:, q * 2048:(q + 1) * 2048], mgu2, self.idxw[:, g, fb * 4 + q: fb * 4 + q + 1],
                                           NE * 128 * 28 - 1), reads=[rR], writes=[wguR[sl][q]], key=wguR[sl][q])
                df = wdn[sl][:].rearrange("p c n -> p (c n)")
                for q in range(2):
                    S.add("pool", i_gather(df[:, q * 2048:(q + 1) * 2048], mdn2, self.idxd[:, g, fb * 2 + q: fb * 2 + q + 1],
                                           NE * 128 * 14 - 1), reads=[rR], writes=[wdnR[sl][q]], key=wdnR[sl][q])

            def load_x(g):
                sl = g % 2
                for ti in range(4):
                    st_ = g * 4 + ti
                    xs = st_ % 2
                    S.add("sp", i_dma(xtm[xs][:], self.xgD[st_ * 128:(st_ + 1) * 128, :]), writes=[xtmR[xs]], key=xtmR[xs])
                    for c in range(8):
                        S.add("pe", i_tr(self.pst[xs][:, c * 128:(c + 1) * 128], xtm[xs][:, c * 128:(c + 1) * 128], self.ident[:]),
                              reads=[xtmR[xs]], writes=[self.pstR[xs]])
                    S.add("act", i_acopy(xgT[sl][:, :, ti * 128:(ti + 1) * 128],
                                          self.pst[xs][:].rearrange("p (c t) -> p c t", c=8)),
                          reads=[self.pstR[xs]], writes=[xgTR[sl][ti]])

            issue_w(0, 0)
            issue_w(0, 1)
            load_x(0)
            cnt = 0
            dcnt = 0
            for g in range(NGRP):
                xsl = g % 2
                xr = xgTR[xsl]
                for fb in range(7):
                    k2 = g * 7 + fb + 2
                    if k2 < NGRP * 7:
                        issue_w(k2 // 7, k2 % 7)
                    if fb == 3 and g + 1 < NGRP:
                        load_x(g + 1)
                    sl = (g * 7 + fb) % NW
                    ysl = (g * 7 + fb) % 2
                    for fc in range(4):
                        pb = cnt % 2
                        cnt += 1
                        for c in range(8):
                            S.add("pe", i_mm(ps[pb][:], wgu[sl][:, c, 0, fc * 128:(fc + 1) * 128], xgT[xsl][:, c, :],
                                              c == 0, c == 7), reads=xr + [wguR[sl][c // 2]], writes=[psR[pb]])
                        for c in range(8):
                            S.add("pe", i_mm(ps[2 + pb][:], wgu[sl][:, c, 1, fc * 128:(fc + 1) * 128], xgT[xsl][:, c, :],
                                              c == 0, c == 7), reads=xr + [wguR[sl][c // 2]], writes=[psR[2 + pb]])
                        S.add("act", i_act(sg[pb][:], ps[pb][:], AF.Silu), reads=[psR[pb]], writes=[sgR[pb]])
                        S.add("dve", i_tt(Y[ysl][:, fc, :], sg[pb][:], ps[2 + pb][:], ALU.mult),
                              reads=[sgR[pb], psR[2 + pb]], writes=[YR[ysl][fc]])
                    for ti in range(4):
                        for hf in range(2):
                            db = 4 + dcnt % 2
                            dcnt += 1
                            for fc in range(4):
                                S.add("pe", i_mm(ps[db][:], Y[ysl][:, fc, ti * 128:(ti + 1) * 128],
                                                  wdn[sl][:, fc, hf * 512:(hf + 1) * 512], fc == 0, fc == 3),
                                      reads=[YR[ysl][fc], wdnR[sl][fc // 2]], writes=[psR[db]])
                            ysl_ = yacc[:, ti, hf * 512:(hf + 1) * 512]
                            if fb == 0:
                                S.add("act", i_acopy(ysl_, ps[db][:]), reads=[psR[db]], writes=[yaccR[ti]])
                            else:
                                S.add("dve", i_tt(ysl_, ps[db][:], ysl_, ALU.add), reads=[psR[db], yaccR[ti]], writes=[yaccR[ti]])
                S.add("sp", i_dma(self.yD[g * 512:(g + 1) * 512, :].rearrange("(a p) c -> p a c", p=128), yacc[:]),
                      reads=yaccR, key=yaccR[0])
            S.end()

    def phase_combine(self):
        S = self.S
        with ExitStack() as es:
            ya = [self.sb(es, [128, D], F32, "ya") for _ in range(2)]
            yb = [self.sb(es, [128, D], F32, "yb") for _ in range(2)]
            yaR = [S.res("ya0"), S.res("ya1")]
            ybR = [S.res("yb0"), S.res("yb1")]
            rR = self.routeR
            outs = []
            for i in range(NT):
                s_ = i % 2
                S.add("pool", i_gather(ya[s_][:], self.yD, self.slotu[:, 0, i:i + 1], NSLOT - 1), reads=[rR], writes=[yaR[s_]], key=yaR[s_])
                S.add("pool", i_gather(yb[s_][:], self.yD, self.slotu[:, 1, i:i + 1], NSLOT - 1), reads=[rR], writes=[ybR[s_]], key=ybR[s_])
                S.add("dve", i_stt(self.h[:, i, :], ya[s_][:], self.p12[:, 0, i:i + 1], self.h[:, i, :], ALU.mult, ALU.add),
                      reads=[yaR[s_], rR, self.hR[i]], writes=[self.hR[i]])
                S.add("dve", i_stt(self.h[:, i, :], yb[s_][:], self.p12[:, 1, i:i + 1], self.h[:, i, :], ALU.mult, ALU.add),
                      reads=[ybR[s_], rR, self.hR[i]], writes=[self.hR[i]])
                outs.append(S.add("sp", i_dma(self.out[i], self.h[:, i, :]), reads=[self.hR[i]], key=self.hR[i]))
            S.end(extra_wait=outs)

    def phase_store(self):
        S = self.S
        outs = []
        for i in range(NT):
            outs.append(S.add("sp", i_dma(self.out[i], self.h[:, i, :]), reads=[self.hR[i]], key=self.hR[i]))
        S.end(extra_wait=outs)


def _prep_gu(Wg, Wu, widths):
    parts = []
    f0 = 0
    for w in widths:
        blk = np.stack([Wg[:, f0:f0 + w], Wu[:, f0:f0 + w]], axis=1)
        blk = blk.reshape(8, 128, 2, w).transpose(1, 0, 2, 3).reshape(128, 16 * w)
        parts.append(blk)
        f0 += w
    return np.ascontiguousarray(np.concatenate(parts, axis=1))


def _prep_dn(Wd):
    nch = Wd.shape[0] // 128
    return np.ascontiguousarray(Wd.reshape(nch, 128, D).transpose(1, 0, 2).reshape(128, nch * D))


def _pk(W):
    return np.ascontiguousarray(W.reshape(8, 128, W.shape[1]).transpose(1, 0, 2))


def _rep(v, n=128):
    return np.ascontiguousarray(np.broadcast_to(np.asarray(v, np.float32)[None, :], (n, v.shape[0])))


def _rope_tables(pos0):
    half = 16
    inv_freq = (np.float32(500000.0) ** (-np.arange(half, dtype=np.float32) / np.float32(half))).astype(np.float32)
    pos = (pos0 + np.arange(T)).astype(np.float32)
    ang = (pos[:, None] * inv_freq[None, :]).astype(np.float32)
    return np.cos(ang).astype(np.float32), np.sin(ang).astype(np.float32)


_CACHE = {}


def _shared_inputs(inp, used):
    f = lambda k: np.asarray(inp[k], np.float32)
    sh = {}
    sh["gn"] = np.stack([_rep(f("e_mix_norm")[0]), _rep(f("e_ffn_norm")[0]), _rep(f("o_mix_norm")[0]), _rep(f("o_ffn_norm")[0])])
    sh["win"] = _pk(f("e_w_in")[0])
    sh["wout"] = _pk(f("e_w_out")[0])
    cw = f("e_conv_w")[0]
    sh["convw"] = np.ascontiguousarray(cw.reshape(3, 4, 128).transpose(2, 1, 0).reshape(128, 12))
    sh["lng"] = np.ascontiguousarray(f("e_gmlp_ln_g")[0].reshape(4, 128).T)
    sh["lnb"] = _rep(f("e_gmlp_ln_b")[0])
    sh["wsT"] = np.ascontiguousarray(f("e_w_spatial")[0].transpose(2, 0, 1))
    bs = f("e_b_spatial")[0]
    bsp = np.zeros((128, 4, 128), np.float32)
    for j in range(4):
        for hf in range(2):
            bsp[hf * 64:(hf + 1) * 64, j, :] = bs[2 * j + hf][None, :]
    sh["bsp"] = bsp.reshape(128, 512)
    sh["tril"] = np.triu(np.ones((128, 128), np.float32))
    sh["egu"] = _prep_gu(f("e_w_gate")[0], f("e_w_up")[0], L0_BLOCKS)
    sh["edn"] = _prep_dn(f("e_w_down")[0])
    sh["wqkv"] = _pk(f("o_w_qkv")[0])
    sh["gq"] = _rep(f("o_q_norm")[0])
    sh["gk"] = _rep(f("o_k_norm")[0])
    sh["wo"] = _pk(f("o_w_o")[0])
    sh["wr"] = _pk(f("o_w_router")[0])
    if "mgu" in used:
        wg, wu, wd = f("o_w_gate")[0], f("o_w_up")[0], f("o_w_down")[0]
        sh["mgu"] = np.stack([_prep_gu(wg[e], wu[e], MOE_BLOCKS) for e in range(NE)])
        sh["mdn"] = np.stack([_prep_dn(wd[e]) for e in range(NE)])
        if SPARSE:
            sh["mgu"] = sh["mgu"].reshape(NE * 128 * 28, 2048)
            sh["mdn"] = sh["mdn"].reshape(NE * 128 * 14, 2048)
    sh["trilb"] = np.triu(np.ones((128, 128), np.float32))
    sh["giota"] = np.ascontiguousarray(np.broadcast_to(np.arange(NGRP, dtype=np.float32)[None, :], (128, NGRP)))
    sh["basew"] = (np.arange(128, dtype=np.float32)[:, None] * 28 + np.arange(28, dtype=np.float32)[None, :]).astype(np.float32)
    sh["based"] = (np.arange(128, dtype=np.float32)[:, None] * 14 + np.arange(14, dtype=np.float32)[None, :]).astype(np.float32)
    sh["ident"] = np.eye(128, dtype=np.float32)
    k = np.arange(128)[:, None]
    q = np.arange(256)[None, :]
    dm = np.concatenate([(k <= q), (128 + k <= q)], axis=1).astype(np.float32)
    sh["dmask"] = dm
    eoh = np.zeros((128, 16, 128), np.float32)
    for n in range(16):
        eoh[n, n, :] = 1.0
    sh["eoh"] = eoh.reshape(128, 2048)
    return sh


def _core_inputs(x, b, half, sh):
    m = dict(sh)
    m["xo"] = np.ascontiguousarray(x[b, half * T:(half + 1) * T].reshape(NT, 128, D))
    if half == 1:
        m["xp"] = np.ascontiguousarray(x[b, 0:T].reshape(NT, 128, D))
    else:
        m["xp"] = np.zeros((NT, 128, D), np.float32)
    cp, sp_ = _rope_tables(0)
    co, so = _rope_tables(half * T)
    rope = np.zeros((128, 2, 32, 16), np.float32)
    rope[:, 0, 0:16, :] = cp.reshape(16, 128, 16).transpose(1, 0, 2)
    rope[:, 1, 0:16, :] = sp_.reshape(16, 128, 16).transpose(1, 0, 2)
    rope[:, 0, 16:32, :] = co.reshape(16, 128, 16).transpose(1, 0, 2)
    rope[:, 1, 16:32, :] = so.reshape(16, 128, 16).transpose(1, 0, 2)
    m["rope"] = rope
    gb = np.full((16, 16), -1e30, np.float32)
    for i in range(16):
        j = i // 2
        if half == 1:
            gb[i, 0:8] = 0.0
        gb[i, 8:8 + j] = 0.0
    m["gbias"] = np.ascontiguousarray(np.broadcast_to(gb.reshape(1, 256), (128, 256)))
    return m


def _run(inputs, stop_after=None):
    x = np.asarray(inputs["x"], np.float32)
    key = stop_after
    if key not in _CACHE:
        bld = Builder(stop_after)
        _CACHE[key] = (bld.build(), set(bld.I.keys()))
    nc, used = _CACHE[key]
    sh = _shared_inputs(inputs, used)
    in_maps = []
    for c in range(8):
        m = _core_inputs(x, c // 2, c % 2, sh)
        in_maps.append({k: v for k, v in m.items() if k in used})
    res = run_bass_kernel_spmd(nc, in_maps, core_ids=list(range(8)))
    out = np.zeros((4, 4096, D), np.float32)
    for c in range(8):
        b, half = c // 2, c % 2
        out[b, half * T:(half + 1) * T] = np.asarray(res.results[c]["out"]).reshape(T, D)
    return out


def kernel(**inputs):
    return _run(inputs)
```

```python
import numpy as np
from contextlib import ExitStack
import concourse.bass as bass
import concourse.mybir as mybir
from concourse.bass_utils import run_bass_kernel_spmd

F32 = mybir.dt.float32
BF16 = mybir.dt.bfloat16
AF = mybir.ActivationFunctionType
ALU = mybir.AluOpType
AX = mybir.AxisListType

D = 1024
T = 2048
NT = 16
NG = 4
EPS = 1e-6
FF0 = 2816
FFE = 3584
NE = 8
L0_BLOCKS = [512, 512, 512, 512, 512, 256]
MOE_BLOCKS = [512] * 7
SCALE = 128 ** -0.5
NDSEM = 90
DBG_NORM = 9
SPARSE = True
NGRP = 15
NSLOT = NGRP * 512
U32 = mybir.dt.uint32
ENGS = ("pe", "act", "dve", "pool", "sp")


class Res:
    __slots__ = ("name", "last_w", "rd", "rd_dma", "last_dma", "sem", "excl")

    def __init__(self, name, excl=False):
        self.name = name
        self.sem = None
        self.excl = excl
        self.reset()

    def reset(self):
        self.last_w = None
        self.rd = {}
        self.rd_dma = []
        self.last_dma = None
        self.sem = None


class Ins:
    __slots__ = ("eng", "fn", "deps", "signal", "sem", "val", "key")

    def __init__(self, eng, fn, key):
        self.eng = eng
        self.fn = fn
        self.key = key
        self.deps = []
        self.signal = False
        self.sem = None
        self.val = 0


class Sched:
    def __init__(self, nc, es):
        self.nc = nc
        self.esem = {e: es.enter_context(nc.semaphore("e_" + e)) for e in ENGS}
        self.ecnt = {e: 0 for e in ENGS}
        self.dsems = [es.enter_context(nc.semaphore("d%d" % i)) for i in range(NDSEM)]
        self.dcnt = [0] * NDSEM
        self.waited = {e: {} for e in ENGS}
        self.allres = []
        self.n_ins = 0
        self.begin()

    def res(self, name, excl=False):
        r = Res(name, excl)
        self.allres.append(r)
        return r

    def begin(self):
        self.q = {e: [] for e in ENGS}
        self.keys = []
        self.free_sw = list(range(0, 30))
        self.free_hw = list(range(30, NDSEM))
        for r in self.allres:
            r.reset()

    def add(self, eng, fn, reads=(), writes=(), key=None):
        ins = Ins(eng, fn, key)
        deps = set()
        xr = [r for r in reads if r.excl]
        if xr:
            writes = list(writes) + xr
            reads = [r for r in reads if not r.excl]
        for r in reads:
            if r.last_w is not None:
                deps.add(r.last_w)
        for w in writes:
            if w.last_w is not None:
                deps.add(w.last_w)
            deps.update(w.rd.values())
            deps.update(w.rd_dma)
        if key is not None and key.last_dma is not None:
            deps.add(key.last_dma)
        final = []
        for d in deps:
            if d is ins:
                continue
            if d.key is None and key is None and d.eng == "pe" and eng == "pe":
                continue
            if d.key is None:
                d.signal = True
            final.append(d)
        ins.deps = final
        for r in reads:
            if key is not None:
                r.rd_dma.append(ins)
            else:
                r.rd[eng] = ins
        for w in writes:
            w.last_w = ins
            w.rd = {}
            w.rd_dma = []
        if key is not None:
            key.last_dma = ins
            if key.sem is None:
                key.sem = (self.free_sw if eng == "pool" else self.free_hw).pop()
                self.keys.append(key)
            self.dcnt[key.sem] += 16
            ins.sem = self.dsems[key.sem]
            ins.val = self.dcnt[key.sem]
        self.q[eng].append(ins)
        self.n_ins += 1
        return ins

    def end(self, extra_wait=()):
        dmas = [k.last_dma for k in self.keys if k.last_dma is not None]
        b = Ins("sp", None, None)
        b.deps = list(dmas) + list(extra_wait)
        self.q["sp"].append(b)
        for e in ENGS:
            for ins in self.q[e]:
                if ins.key is None and ins.signal:
                    self.ecnt[e] += 1
                    ins.sem = self.esem[e]
                    ins.val = self.ecnt[e]
        nc = self.nc
        sched = self

        def mk(e):
            def f(eng):
                waited = sched.waited[e]
                for ins in sched.q[e]:
                    for d in ins.deps:
                        sid = id(d.sem)
                        if waited.get(sid, -1) < d.val:
                            eng.wait_ge(d.sem, d.val)
                            waited[sid] = d.val
                    if ins.fn is None:
                        continue
                    r = ins.fn(eng)
                    if ins.key is not None:
                        r.then_inc(ins.sem, 16)
                    elif ins.signal:
                        r.then_inc(ins.sem, 1)
            return f

        _BOUND_REGS.clear()
        with nc.Block() as block:
            block.tensor(mk("pe"))
            block.scalar(mk("act"))
            block.vector(mk("dve"))
            block.gpsimd(mk("pool"))
            block.sync(mk("sp"))
        self.begin()


class _Borrow:
    def __init__(self, es):
        self.es = es

    def __enter__(self):
        return self.es

    def __exit__(self, *a):
        return False


def i_mm(out, lhsT, rhs, start, stop, skip=False):
    if skip:
        return lambda e: e.matmul(out, lhsT, rhs, start=start, stop=stop, skip_group_check=True)
    return lambda e: e.matmul(out, lhsT, rhs, start=start, stop=stop)


def i_tr(out, in_, ident):
    return lambda e: e.transpose(out, in_, ident)


def i_act(out, in_, func, bias=None, scale=None, accum_out=None):
    kw = {}
    if bias is not None:
        kw["bias"] = bias
    if scale is not None:
        kw["scale"] = scale
    if accum_out is not None:
        kw["accum_out"] = accum_out
    return lambda e: e.activation(out=out, in_=in_, func=func, **kw)


def i_ts(out, in0, s1, s2, op0, op1=None):
    if op1 is None:
        return lambda e: e.tensor_scalar(out=out, in0=in0, scalar1=s1, scalar2=None, op0=op0)
    return lambda e: e.tensor_scalar(out=out, in0=in0, scalar1=s1, scalar2=s2, op0=op0, op1=op1)


def i_tt(out, in0, in1, op):
    return lambda e: e.tensor_tensor(out=out, in0=in0, in1=in1, op=op)


def i_stt(out, in0, scalar, in1, op0, op1):
    return lambda e: e.scalar_tensor_tensor(out=out, in0=in0, scalar=scalar, in1=in1, op0=op0, op1=op1)


def i_copy(out, in_):
    return lambda e: e.tensor_copy(out=out, in_=in_)


def i_acopy(out, in_):
    return lambda e: e.activation(out=out, in_=in_, func=AF.Identity)


def i_dma(out, in_):
    return lambda e: e.dma_start(out=out, in_=in_)


_BOUND_REGS = {}


def _bound_reg(e, bound):
    r = _BOUND_REGS.get(bound)
    if r is None:
        r = e.to_reg(bound)
        _BOUND_REGS[bound] = r
    return r


def i_gather(out, in_, idx, bound):
    return lambda e: e.indirect_dma_start(out=out, out_offset=None, in_=in_,
                                          in_offset=bass.IndirectOffsetOnAxis(ap=idx, axis=0),
                                          bounds_check=_bound_reg(e, bound), oob_is_err=False)


def i_scatter(out, idx, in_, bound):
    return lambda e: e.indirect_dma_start(out=out, out_offset=bass.IndirectOffsetOnAxis(ap=idx, axis=0),
                                          in_=in_, in_offset=None, bounds_check=_bound_reg(e, bound), oob_is_err=False)


def i_memset(ap, v):
    return lambda e: e.memset(ap, v)


def i_red(out, in_, op, axis=AX.X, absval=None):
    if absval:
        return lambda e: e.tensor_reduce(out=out, in_=in_, axis=axis, op=op, apply_absolute_value=True)
    return lambda e: e.tensor_reduce(out=out, in_=in_, axis=axis, op=op)


def i_max8(out, in_):
    return lambda e: e.max(out=out, in_=in_)


def i_bnstats(out, in_):
    return lambda e: e.bn_stats(out=out, in_=in_)


def i_bnaggr(out, in_):
    return lambda e: e.bn_aggr(out=out, in_=in_)


def i_recip(out, in_):
    return lambda e: e.reciprocal(out=out, in_=in_)


class Builder:
    def __init__(self, stop_after=None):
        self.stop_after = stop_after
        self.nc = bass.Bass("TRN2", target_bir_lowering=False)
        self.uid = 0

    def name(self, p):
        self.uid += 1
        return "%s_%d" % (p, self.uid)

    def din(self, name, shape, dt=F32):
        return self.nc.dram_tensor(name, list(shape), dt, kind="ExternalInput").ap()

    def sb(self, es, shape, dt, p="t"):
        return es.enter_context(self.nc.sbuf_tensor(self.name(p), list(shape), dt))

    def build(self):
        nc = self.nc
        shapes = {
            "xo": [NT, 128, D], "xp": [NT, 128, D], "gn": [4, 128, D], "win": [128, 8, 2560], "wout": [128, 8, D],
            "convw": [128, 12], "lng": [128, 4], "lnb": [128, 512], "wsT": [128, 8, 128], "bsp": [128, 512],
            "tril": [128, 128], "egu": [128, 8 * 2 * FF0], "edn": [128, (FF0 // 128) * D], "wqkv": [128, 8, 3072],
            "gq": [128, 128], "gk": [128, 128], "wo": [128, 8, D], "wr": [128, 8, 8],
            "mgu": ([NE * 128 * 28, 2048] if SPARSE else [NE, 128, 8 * 2 * FFE]), "mdn": ([NE * 128 * 14, 2048] if SPARSE else [NE, 128, (FFE // 128) * D]),
            "trilb": [128, 128], "giota": [128, NGRP], "basew": [128, 28], "based": [128, 14], "rope": [128, 2, 32, 16],
            "ident": [128, 128], "dmask": [128, 512], "eoh": [128, 2048], "gbias": [128, 256],
        }
        bld = self

        class LazyIn(dict):
            def __missing__(self, k):
                v = bld.din(k, shapes[k])
                self[k] = v
                return v
        I = LazyIn()
        self.I = I
        self.out = nc.dram_tensor("out", [NT, 128, D], F32, kind="ExternalOutput").ap()
        self.Qs = nc.dram_tensor("Qs", [8, 128, T], BF16, kind="Internal").ap()
        self.Ks = nc.dram_tensor("Ks", [8, 128, 2 * T], BF16, kind="Internal").ap()
        self.Vs = nc.dram_tensor("Vs", [8, 2 * NT, 128, 128], BF16, kind="Internal").ap()

        with ExitStack() as es:
            self.S = S = Sched(nc, es)
            self.h = self.sb(es, [128, NT, D], F32, "h")
            self.hR = [S.res("h%d" % i) for i in range(NT)]
            self.hnTR = [S.res("hnT%d" % i) for i in range(NT)]
            self.gnorm = self.sb(es, [128, D], F32, "gnorm")
            self.gnormR = S.res("gnorm")
            self.ident = self.sb(es, [128, 128], BF16, "ident")
            self.ones = self.sb(es, [128, 128], BF16, "ones")
            self.rope = self.sb(es, [128, 2, 32, 16], F32, "rope")
            self.convw = self.sb(es, [128, 12], F32, "convw")
            self.lng = self.sb(es, [128, 4], F32, "lng")
            self.WcT = self.sb(es, [128, 8, 128], BF16, "WcT")
            self.biasf = self.sb(es, [128, 4, 128], F32, "biasf")
            self.halo = self.sb(es, [128, 4, 2], F32, "halo")
            self.gq = self.sb(es, [128, 128], F32, "gq")
            self.gk = self.sb(es, [128, 128], F32, "gk")
            self.negB = self.sb(es, [128, 1], F32, "negB")
            self.dmask = self.sb(es, [128, 512], BF16, "dmask")
            self.gbias = self.sb(es, [128, 256], F32, "gbias")
            self.gates = self.sb(es, [128, NT, 8], F32, "gates")
            self.constR = S.res("consts")
            self.haloR = S.res("halo")
            self.gatesR = S.res("gates")
            self.ps = [es.enter_context(nc.psum_tensor("ps%d" % i, [128, 512], F32)) for i in range(6)]
            self.psR = [S.res("ps%d" % i, excl=True) for i in range(6)]
            self.pst = [es.enter_context(nc.psum_tensor("pst%d" % i, [128, 1024], BF16)) for i in range(2)]
            self.pstR = [S.res("pst0", excl=True), S.res("pst1", excl=True)]

            self.p12 = self.sb(es, [128, 2, NT], F32, "p12")
            self.slotu = self.sb(es, [128, 2, NT], U32, "slotu")
            self.idxw = self.sb(es, [128, NGRP, 28], U32, "idxw")
            self.idxd = self.sb(es, [128, NGRP, 14], U32, "idxd")
            self.trilb = self.sb(es, [128, 128], BF16, "trilb")
            self.giota = self.sb(es, [128, NGRP], F32, "giota")
            self.basew = self.sb(es, [128, 28], F32, "basew")
            self.based = self.sb(es, [128, 14], F32, "based")
            self.routeR = S.res("route")
            self.xgD = nc.dram_tensor("xgD", [NSLOT, D], BF16, kind="Internal").ap()
            self.yD = nc.dram_tensor("yD", [NSLOT, D], F32, kind="Internal").ap()
            es_hn = ExitStack()
            self.hnT = self.sb(es_hn, [128, 8, T], BF16, "hnT")

            self.phase_setup()
            steps = []
            for seg in (0, 1):
                steps += [("norm0", seg), ("mix", seg), ("norm1", seg), ("ffn", seg), ("norm2", seg), ("qkv", seg)]
            steps += [("attn", 1), ("norm3", 1), ("moe", 1)]
            stop = self.stop_after
            if stop != "setup":
                for (st, seg) in steps:
                    if st == "norm0":
                        self.phase_norm(0, load=("xp" if seg == 0 else "xo"))
                    elif st == "mix":
                        self.phase_mix(seg)
                    elif st == "norm1":
                        self.phase_norm(1)
                    elif st == "ffn":
                        self.phase_ffn(I["egu"], I["edn"], [L0_BLOCKS], None)
                    elif st == "norm2":
                        self.phase_norm(2)
                    elif st == "qkv":
                        self.phase_qkv(seg)
                    elif st == "attn":
                        self.phase_attn()
                    elif st == "norm3":
                        self.phase_norm(3, router=True)
                    elif st == "moe":
                        if SPARSE:
                            es_hn.close()
                            self.phase_moe_sparse()
                            return nc
                        self.phase_ffn(I["mgu"], I["mdn"], [MOE_BLOCKS] * NE, self.gates)
                    if stop == "%s_%d" % (st, seg):
                        break
            es_hn.close()
            self.phase_store()
        return nc

    def phase_setup(self):
        S, I = self.S, self.I
        with ExitStack() as es:
            cR = self.constR
            tmpw = self.sb(es, [128, 8, 128], F32, "tmpw")
            tril = self.sb(es, [128, 128], F32, "tril")
            bmat = self.sb(es, [128, 512], BF16, "bmat")
            bsp = self.sb(es, [128, 512], F32, "bsp")
            mq = self.sb(es, [128, 2], F32, "mq")
            rs = [S.res("su%d" % i) for i in range(16)]
            S.add("sp", i_dma(self.rope[:], I["rope"]), writes=[rs[0]], key=rs[0])
            S.add("sp", i_dma(self.convw[:], I["convw"]), writes=[rs[1]], key=rs[1])
            S.add("sp", i_dma(self.lng[:], I["lng"]), writes=[rs[2]], key=rs[2])
            S.add("sp", i_dma(self.gq[:], I["gq"]), writes=[rs[3]], key=rs[3])
            S.add("sp", i_dma(self.gk[:], I["gk"]), writes=[rs[4]], key=rs[4])
            S.add("sp", i_dma(self.gbias[:], I["gbias"]), writes=[rs[5]], key=rs[5])
            S.add("sp", i_dma(tmpw[:], I["wsT"]), writes=[rs[6]], key=rs[6])
            S.add("sp", i_dma(tril[:], I["tril"]), writes=[rs[7]], key=rs[7])
            S.add("sp", i_dma(bsp[:], I["bsp"]), writes=[rs[8]], key=rs[8])
            S.add("pool", i_dma(self.ident[:], I["ident"]), writes=[rs[9]], key=rs[9])
            S.add("pool", i_dma(self.dmask[:], I["dmask"]), writes=[rs[10]], key=rs[10])
            S.add("pool", i_dma(bmat[:], I["lnb"]), writes=[rs[12]], key=rs[12])
            S.add("dve", i_memset(self.ones[:], 1.0), writes=[rs[13]])
            if SPARSE:
                rx = [S.res("sx%d" % i) for i in range(4)]
                S.add("pool", i_dma(self.trilb[:], I["trilb"]), writes=[rx[0]], key=rx[0])
                S.add("sp", i_dma(self.giota[:], I["giota"]), writes=[rx[1]], key=rx[1])
                S.add("sp", i_dma(self.basew[:], I["basew"]), writes=[rx[2]], key=rx[2])
                S.add("sp", i_dma(self.based[:], I["based"]), writes=[rx[3]], key=rx[3])
            S.add("dve", i_memset(self.halo[:], 0.0), writes=[self.haloR])
            S.add("dve", i_red(mq[:, 0:1], self.gq[:], ALU.max, absval=True), reads=[rs[3]], writes=[rs[14]])
            S.add("dve", i_red(mq[:, 1:2], self.gk[:], ALU.max, absval=True), reads=[rs[4]], writes=[rs[15]])
            S.add("dve", i_ts(self.negB[:], mq[:, 0:1], mq[:, 1:2], -SCALE * 128.0, ALU.mult, ALU.mult),
                  reads=[rs[14], rs[15]], writes=[cR])
            S.add("dve", i_ts(self.gq[:], self.gq[:], 128.0 ** 0.5, None, ALU.mult), reads=[rs[14]], writes=[rs[3]])
            S.add("dve", i_ts(self.gk[:], self.gk[:], 128.0 ** 0.5, None, ALU.mult), reads=[rs[15]], writes=[rs[4]])
            S.add("dve", i_tt(self.WcT[:], tmpw[:], tril[:].unsqueeze(1).to_broadcast([128, 8, 128]), ALU.mult),
                  reads=[rs[6], rs[7]], writes=[rs[6]])
            for j in range(4):
                for hf in range(2):
                    g = 2 * j + hf
                    S.add("pe", i_mm(self.ps[0][hf * 64:(hf + 1) * 64, j * 128:(j + 1) * 128],
                                      bmat[:, g * 64:(g + 1) * 64], self.WcT[:, g, :], True, True),
                          reads=[rs[12], rs[6]], writes=[self.psR[0]])
            S.add("dve", i_tt(self.biasf[:].rearrange("p j t -> p (j t)"), self.ps[0][:], bsp[:], ALU.add),
                  reads=[self.psR[0], rs[8]], writes=[cR])
            S.end()

    def phase_norm(self, gi, load=None, router=False):
        S, I = self.S, self.I
        with ExitStack() as es:
            junk = [self.sb(es, [128, D], BF16, "junk") for _ in range(2)]
            junkR = [S.res("junk0"), S.res("junk1")]
            if router and SPARSE:
                hn_all = self.sb(es, [128, NT, D], BF16, "hn_all")
                hn = [hn_all[:, i, :] for i in range(NT)]
                hnR = [S.res("hna%d" % i) for i in range(NT)]
            else:
                hn2 = [self.sb(es, [128, D], BF16, "hn") for _ in range(2)]
                hn = [hn2[i % 2][:] for i in range(NT)]
                hnR2 = [S.res("hn0"), S.res("hn1")]
                hnR = [hnR2[i % 2] for i in range(NT)]
            ss = self.sb(es, [128, NT], F32, "ss")
            rsd = self.sb(es, [128, NT], F32, "rsd")
            ssR = [S.res("ss%d" % i) for i in range(NT)]
            rsR = [S.res("rs%d" % i) for i in range(NT)]
            S.add("sp", i_dma(self.gnorm[:], I["gn"][gi]), writes=[self.gnormR], key=self.gnormR)
            S.add("dve", i_ts(self.gnorm[:], self.gnorm[:], 32.0, None, ALU.mult), reads=[self.gnormR], writes=[self.gnormR])
            if load is not None:
                for i in range(NT):
                    S.add("sp", i_dma(self.h[:, i, :], I[load][i]), writes=[self.hR[i]], key=self.hR[i])
            if router:
                wr = self.sb(es, [128, 8, 8], BF16, "wr")
                wrR = S.res("wr")
                S.add("pool", i_dma(wr[:], I["wr"]), writes=[wrR], key=wrR)
            S.add("dve", i_memset(ss[:], 0.0), writes=ssR)
            for i in range(NT):
                sl = i % 2
                S.add("act", i_act(junk[sl][:], self.h[:, i, :], AF.Square, accum_out=ss[:, i:i + 1]),
                      reads=[self.hR[i]], writes=[junkR[sl], ssR[i]])
            S.add("act", i_act(rsd[:], ss[:], AF.Sqrt, bias=D * EPS, scale=1.0), reads=ssR, writes=rsR)
            S.add("dve", i_recip(rsd[:], rsd[:]), reads=rsR, writes=rsR)
            for i in range(NT):
                sl = i % 2
                S.add("dve", i_stt(hn[i], self.h[:, i, :], rsd[:, i:i + 1], self.gnorm[:], ALU.mult, ALU.mult),
                      reads=[self.hR[i], rsR[i], self.gnormR], writes=[hnR[i]])
                for c in range(8):
                    S.add("pe", i_tr(self.pst[sl][:, c * 128:(c + 1) * 128], hn[i][:, c * 128:(c + 1) * 128], self.ident[:]),
                          reads=[hnR[i]], writes=[self.pstR[sl]])
                S.add("act", i_acopy(self.hnT[:, :, i * 128:(i + 1) * 128],
                                      self.pst[sl][:].rearrange("p (c t) -> p c t", c=8)),
                      reads=[self.pstR[sl]], writes=[self.hnTR[i]])
            if router and SPARSE:
                self.router_sparse(es, wr, wrR, hn, hnR)
            elif router:
                self.router(es, wr, wrR)
            S.end()

    def router(self, es, wr, wrR):
        S = self.S
        lg = self.sb(es, [128, NT, 8], F32, "lg")
        mx = self.sb(es, [128, NT, 8], F32, "mx")
        tmp = self.sb(es, [128, 4, NT], F32, "rtmp")
        eq = self.sb(es, [128, 2, NT, 8], F32, "eq")
        lgR, mxR, tR, eR = S.res("lg"), S.res("mx"), S.res("rtmp"), S.res("eq")
        psr = self.ps[0]
        for i in range(NT):
            for c in range(8):
                S.add("pe", i_mm(psr[:, i * 8:(i + 1) * 8], self.hnT[:, c, i * 128:(i + 1) * 128], wr[:, c, :],
                                  c == 0, c == 7), reads=[self.hnTR[i], wrR], writes=[self.psR[0]])
        S.add("dve", i_copy(lg[:].rearrange("p t e -> p (t e)"), psr[:, 0:NT * 8]), reads=[self.psR[0]], writes=[lgR])
        for i in range(NT):
            S.add("dve", i_max8(mx[:, i, :], lg[:, i, :]), reads=[lgR], writes=[mxR])
        S.add("dve", i_tt(tmp[:, 0, :], mx[:, :, 1], mx[:, :, 0], ALU.subtract), reads=[mxR], writes=[tR])
        S.add("act", i_act(tmp[:, 1, :], tmp[:, 0, :], AF.Exp), reads=[tR], writes=[tR])
        S.add("dve", i_ts(tmp[:, 2, :], tmp[:, 1, :], 1.0, None, ALU.add), reads=[tR], writes=[tR])
        S.add("dve", i_recip(tmp[:, 2, :], tmp[:, 2, :]), reads=[tR], writes=[tR])
        S.add("dve", i_tt(tmp[:, 3, :], tmp[:, 1, :], tmp[:, 2, :], ALU.mult), reads=[tR], writes=[tR])
        for k in range(2):
            S.add("dve", i_tt(eq[:, k, :, :], lg[:], mx[:, :, k:k + 1].to_broadcast([128, NT, 8]), ALU.is_equal),
                  reads=[lgR, mxR], writes=[eR])
            S.add("dve", i_tt(eq[:, k, :, :], eq[:, k, :, :], tmp[:, 2 + k, :].unsqueeze(2).to_broadcast([128, NT, 8]), ALU.mult),
                  reads=[eR, tR], writes=[eR])
        S.add("dve", i_tt(self.gates[:], eq[:, 0, :, :], eq[:, 1, :, :], ALU.add), reads=[eR], writes=[self.gatesR])

    def phase_mix(self, seg):
        S, I = self.S, self.I
        with ExitStack() as es:
            win = self.sb(es, [128, 8, 2560], BF16, "win")
            winR = [S.res("win%d" % k) for k in range(8)]
            wout = self.sb(es, [128, 8, D], BF16, "wout")
            woutR = [S.res("wout%d" % k) for k in range(2)]
            ycat = [self.sb(es, [128, 8, 512], BF16, "ycat") for _ in range(2)]
            ycR = [[S.res("yc%d_%d" % (s, f)) for f in range(8)] for s in range(2)]
            vhat = [self.sb(es, [128, 512], BF16, "vhat") for _ in range(4)]
            vhR = [S.res("vh%d" % i) for i in range(4)]
            zin = self.sb(es, [128, 4, 514], F32, "zin")
            zR = [S.res("zin%d" % j) for j in range(4)]
            ah = self.sb(es, [128, 512], F32, "ah")
            ahR = S.res("ah")
            t1 = self.sb(es, [128, 512], F32, "t1")
            t1R = S.res("t1")
            tmpb = self.sb(es, [128, 512], F32, "tmpb")
            tbR = S.res("tmpb")
            st = self.sb(es, [128, 4, 6], F32, "bnst")
            mv = self.sb(es, [128, 4, 4], F32, "bnmv")
            stR = [S.res("st%d" % i) for i in range(4)]
            for k in range(8):
                S.add("pool", i_dma(win[:, k, :], I["win"][:, k, :]), writes=[winR[k]], key=winR[k])
            for k in range(2):
                S.add("pool", i_dma(wout[:, 4 * k:4 * k + 4, :], I["wout"][:, 4 * k:4 * k + 4, :]), writes=[woutR[k]], key=woutR[k])
            S.add("dve", i_copy(zin[:, :, 0:2], self.halo[:]), reads=[self.haloR], writes=zR)
            cR = self.constR
            ps, psR = self.ps, self.psR
            for g in range(NG):
                ys, yR = ycat[g % 2], ycR[g % 2]
                gcols = slice(g * 512, (g + 1) * 512)
                gt = [self.hnTR[4 * g + ti] for ti in range(4)]
                for ti in range(4):
                    tile = 4 * g + ti
                    b = ti % 2
                    for c in range(8):
                        S.add("pe", i_mm(ps[b][:], self.hnT[:, c, tile * 128:(tile + 1) * 128], win[:, c, 2048:2560],
                                          c == 0, c == 7), reads=[self.hnTR[tile], winR[c]], writes=[psR[b]])
                    S.add("dve", i_bnstats(st[:, ti, :], ps[b][:]), reads=[psR[b]], writes=[stR[ti]])
                    S.add("dve", i_bnaggr(mv[:, ti, 0:2], st[:, ti, :]), reads=[stR[ti]], writes=[stR[ti]])
                    S.add("act", i_act(mv[:, ti, 2:3], mv[:, ti, 1:2], AF.Sqrt, bias=EPS, scale=1.0), reads=[stR[ti]], writes=[stR[ti]])
                    S.add("dve", i_recip(mv[:, ti, 2:3], mv[:, ti, 2:3]), reads=[stR[ti]], writes=[stR[ti]])
                    S.add("dve", i_ts(mv[:, ti, 3:4], mv[:, ti, 0:1], mv[:, ti, 2:3], -1.0, ALU.mult, ALU.mult),
                          reads=[stR[ti]], writes=[stR[ti]])
                    S.add("act", i_act(vhat[ti][:], ps[b][:], AF.Identity, bias=mv[:, ti, 3:4], scale=mv[:, ti, 2:3]),
                          reads=[psR[b], stR[ti]], writes=[vhR[ti]])
                for j in range(4):
                    for ti in range(4):
                        for hf in range(2):
                            gg = 2 * j + hf
                            S.add("pe", i_mm(ps[2][hf * 64:(hf + 1) * 64, ti * 128:(ti + 1) * 128],
                                              vhat[ti][:, gg * 64:(gg + 1) * 64], self.WcT[:, gg, :], True, True),
                                  reads=[vhR[ti], cR], writes=[psR[2]])
                    for c in range(8):
                        S.add("pe", i_mm(ps[3][:], win[:, c, 1536 + j * 128:1536 + (j + 1) * 128], self.hnT[:, c, gcols],
                                          c == 0, c == 7), reads=gt + [winR[c]], writes=[psR[3]])
                    S.add("dve", i_stt(tmpb[:].rearrange("p (a t) -> p a t", a=4), ps[2][:].rearrange("p (a t) -> p a t", a=4),
                                        self.lng[:, j:j + 1],
                                        self.biasf[:, j, :].unsqueeze(1).to_broadcast([128, 4, 128]), ALU.mult, ALU.add),
                          reads=[psR[2], cR], writes=[tbR])
                    S.add("dve", i_tt(ys[:, 4 + j, :], tmpb[:], ps[3][:], ALU.mult), reads=[tbR, psR[3]], writes=[yR[4 + j]])
                for j in range(4):
                    for part in range(3):
                        for c in range(8):
                            S.add("pe", i_mm(ps[4 + part % 2][:], win[:, c, part * 512 + j * 128: part * 512 + (j + 1) * 128],
                                              self.hnT[:, c, gcols], c == 0, c == 7),
                                  reads=gt + [winR[c]], writes=[psR[4 + part % 2]])
                        if part == 0:
                            S.add("act", i_acopy(ah[:], ps[4][:]), reads=[psR[4]], writes=[ahR])
                        elif part == 1:
                            S.add("dve", i_tt(zin[:, j, 2:514], ps[5][:], ah[:], ALU.mult), reads=[psR[5], ahR], writes=[zR[j]])
                    S.add("dve", i_ts(t1[:], zin[:, j, 2:514], self.convw[:, 3 * j + 2:3 * j + 3], None, ALU.mult),
                          reads=[zR[j], cR], writes=[t1R])
                    S.add("dve", i_stt(t1[:], zin[:, j, 1:513], self.convw[:, 3 * j + 1:3 * j + 2], t1[:], ALU.mult, ALU.add),
                          reads=[zR[j], t1R], writes=[t1R])
                    S.add("dve", i_stt(t1[:], zin[:, j, 0:512], self.convw[:, 3 * j:3 * j + 1], t1[:], ALU.mult, ALU.add),
                          reads=[zR[j], t1R], writes=[t1R])
                    S.add("dve", i_tt(ys[:, j, :], ps[4][:], t1[:], ALU.mult), reads=[psR[4], t1R], writes=[yR[j]])
                    S.add("dve", i_copy(zin[:, j, 0:2], zin[:, j, 512:514]), reads=[zR[j]], writes=[zR[j]])
                for ti in range(4):
                    tile = 4 * g + ti
                    for hf in range(2):
                        b = hf
                        for f in range(8):
                            S.add("pe", i_mm(ps[b][:], ys[:, f, ti * 128:(ti + 1) * 128], wout[:, f, hf * 512:(hf + 1) * 512],
                                              f == 0, f == 7), reads=[yR[f], woutR[f // 4]], writes=[psR[b]])
                        S.add("dve", i_tt(self.h[:, tile, hf * 512:(hf + 1) * 512], ps[b][:], self.h[:, tile, hf * 512:(hf + 1) * 512], ALU.add),
                              reads=[psR[b], self.hR[tile]], writes=[self.hR[tile]])
            S.add("dve", i_copy(self.halo[:], zin[:, :, 512:514]), reads=zR, writes=[self.haloR])
            S.end()

    def phase_ffn(self, gu_d, dn_d, expert_blocks, gates):
        S = self.S
        ne = len(expert_blocks)
        with ExitStack() as es:
            wgu = [self.sb(es, [128, 8, 2, 512], BF16, "wgu") for _ in range(2)]
            wdn = [self.sb(es, [128, 4, D], BF16, "wdn") for _ in range(2)]
            wguR = [S.res("wgu0"), S.res("wgu1")]
            wdnR = [S.res("wdn0"), S.res("wdn1")]
            Y = [self.sb(es, [128, 4, 512], BF16, "Y") for _ in range(2)]
            YR = [[S.res("Y%d_%d" % (s, f)) for f in range(4)] for s in range(2)]
            sg = [self.sb(es, [128, 512], F32, "sg") for _ in range(2)]
            sgR = [S.res("sg0"), S.res("sg1")]
            ps, psR = self.ps, self.psR
            blocks = []
            for e in range(ne):
                f0 = 0
                for w in expert_blocks[e]:
                    blocks.append((e, f0, w))
                    f0 += w
            def issue(bi):
                e, f0, w = blocks[bi]
                sl = bi % 2
                nch = w // 128
                if ne == 1:
                    src_gu = gu_d[:, 16 * f0:16 * (f0 + w)]
                    src_dn = dn_d[:, (f0 // 128) * D:(f0 // 128 + nch) * D]
                else:
                    src_gu = gu_d[e, :, 16 * f0:16 * (f0 + w)]
                    src_dn = dn_d[e, :, (f0 // 128) * D:(f0 // 128 + nch) * D]
                S.add("pool", i_dma(wgu[sl][:, :, :, 0:w], src_gu.rearrange("p (k g f) -> p k g f", k=8, g=2)),
                      writes=[wguR[sl]], key=wguR[sl])
                S.add("pool", i_dma(wdn[sl][:, 0:nch, :], src_dn.rearrange("p (c n) -> p c n", c=nch)),
                      writes=[wdnR[sl]], key=wdnR[sl])
            issue(0)
            cnt = 0
            dcnt = 0
            for bi, (e, f0, w) in enumerate(blocks):
                if bi + 1 < len(blocks):
                    issue(bi + 1)
                sl = bi % 2
                nch = w // 128
                for g in range(NG):
                    gcols = slice(g * 512, (g + 1) * 512)
                    gt = [self.hnTR[4 * g + ti] for ti in range(4)]
                    ysl = (bi * NG + g) % 2
                    for fc in range(nch):
                        pb = cnt % 2
                        cnt += 1
                        for c in range(8):
                            S.add("pe", i_mm(ps[pb][:], wgu[sl][:, c, 0, fc * 128:(fc + 1) * 128], self.hnT[:, c, gcols],
                                              c == 0, c == 7), reads=gt + [wguR[sl]], writes=[psR[pb]])
                        for c in range(8):
                            S.add("pe", i_mm(ps[2 + pb][:], wgu[sl][:, c, 1, fc * 128:(fc + 1) * 128], self.hnT[:, c, gcols],
                                              c == 0, c == 7), reads=gt + [wguR[sl]], writes=[psR[2 + pb]])
                        S.add("act", i_act(sg[pb][:], ps[pb][:], AF.Silu), reads=[psR[pb]], writes=[sgR[pb]])
                        S.add("dve", i_tt(Y[ysl][:, fc, :], sg[pb][:], ps[2 + pb][:], ALU.mult),
                              reads=[sgR[pb], psR[2 + pb]], writes=[YR[ysl][fc]])
                    for ti in range(4):
                        tile = 4 * g + ti
                        for hf in range(2):
                            db = 4 + dcnt % 2
                            dcnt += 1
                            for fc in range(nch):
                                S.add("pe", i_mm(ps[db][:], Y[ysl][:, fc, ti * 128:(ti + 1) * 128],
                                                  wdn[sl][:, fc, hf * 512:(hf + 1) * 512], fc == 0, fc == nch - 1),
                                      reads=[YR[ysl][fc], wdnR[sl]], writes=[psR[db]])
                            hsl = self.h[:, tile, hf * 512:(hf + 1) * 512]
                            if gates is None:
                                S.add("dve", i_tt(hsl, ps[db][:], hsl, ALU.add),
                                      reads=[psR[db], self.hR[tile]], writes=[self.hR[tile]])
                            else:
                                S.add("dve", i_stt(hsl, ps[db][:], gates[:, tile, e:e + 1], hsl, ALU.mult, ALU.add),
                                      reads=[psR[db], self.hR[tile], self.gatesR], writes=[self.hR[tile]])
            S.end()

    def phase_qkv(self, seg):
        S, I = self.S, self.I
        with ExitStack() as es:
            NS = 3
            wq = [self.sb(es, [128, 8, 512], BF16, "wq") for _ in range(2)]
            wqR = [S.res("wq0"), S.res("wq1")]
            sq = [self.sb(es, [128, 128], BF16, "sq") for _ in range(2)]
            sqR = [S.res("sq0"), S.res("sq1")]
            ssq = [self.sb(es, [128, 2, 4], F32, "ssq") for _ in range(NS)]
            ssR = [S.res("ssq%d" % i) for i in range(NS)]
            qn = [self.sb(es, [128, 4, 128], F32, "qn") for _ in range(NS)]
            qnR = [S.res("qn%d" % i) for i in range(NS)]
            rt = [self.sb(es, [128, 4, 4, 16], F32, "rt") for _ in range(NS)]
            rtR = [S.res("rt%d" % i) for i in range(NS)]
            NQ = 4
            qb = [self.sb(es, [128, 4, 128], BF16, "qb") for _ in range(NQ)]
            qbR = [S.res("qb%d" % i) for i in range(NQ)]
            stg = [self.sb(es, [128, 4, 512], BF16, "stg") for _ in range(2)]
            stgR = [S.res("stg0"), S.res("stg1")]
            vst = [self.sb(es, [128, 4, 128], BF16, "vst") for _ in range(2)]
            vstR = [S.res("vst0"), S.res("vst1")]
            ps, psR = self.ps, self.psR
            cR = self.constR
            cbs = ([0, 1] if seg == 1 else []) + [2, 3, 4, 5]

            def issue(ci):
                cb = cbs[ci]
                S.add("pool", i_dma(wq[ci % 2][:], I["wqkv"][:, :, cb * 512:(cb + 1) * 512]), writes=[wqR[ci % 2]], key=wqR[ci % 2])
            issue(0)
            cnt = 0
            state = {"gcount": 0, "tcount": 0}
            pending = []

            def make_post(kind, hd0, i, qs):
                def post():
                    tb = state["tcount"] % 2
                    state["tcount"] += 1
                    for hh in range(4):
                        S.add("pe", i_tr(self.pst[tb][:, hh * 128:(hh + 1) * 128], qb[qs][:, hh, :], self.ident[:]),
                              reads=[qbR[qs], cR], writes=[self.pstR[tb]])
                    ss_ = state["gcount"] % 2
                    ti = i % 4
                    S.add("act", i_acopy(stg[ss_][:, :, ti * 128:(ti + 1) * 128],
                                          self.pst[tb][:, 0:512].rearrange("p (h t) -> p h t", h=4)),
                          reads=[self.pstR[tb]], writes=[stgR[ss_]])
                    if ti == 3:
                        g = i // 4
                        if kind == 0:
                            dst = self.Qs[hd0:hd0 + 4, :, g * 512:(g + 1) * 512]
                        else:
                            dst = self.Ks[hd0:hd0 + 4, :, seg * T + g * 512: seg * T + (g + 1) * 512]
                        S.add("sp", i_dma(dst.rearrange("h d t -> d h t"), stg[ss_][:]), reads=[stgR[ss_]], key=stgR[ss_])
                        state["gcount"] += 1
                return post

            pendB = []

            def make_B(kind, pb, s_, qs, gtile):
                gvec = self.gq if kind == 0 else self.gk
                r_ = rt[s_]
                x1 = qn[s_][:, :, 0:16]
                x2 = qn[s_][:, :, 16:32]
                cosb = self.rope[:, 0, gtile, :].unsqueeze(1).to_broadcast([128, 4, 16])
                sinb = self.rope[:, 1, gtile, :].unsqueeze(1).to_broadcast([128, 4, 16])

                def part1():
                    for hh in range(4):
                        S.add("dve", i_stt(qn[s_][:, hh, :], ps[pb][:, hh * 128:(hh + 1) * 128], ssq[s_][:, 1, hh:hh + 1], gvec[:], ALU.mult, ALU.mult),
                              reads=[psR[pb], ssR[s_], cR], writes=[qnR[s_]])
                    S.add("act", i_acopy(qb[qs][:], qn[s_][:]), reads=[qnR[s_]], writes=[qbR[qs]])
                    S.add("dve", i_tt(r_[:, 0, :, :], x1, cosb, ALU.mult), reads=[qnR[s_], cR], writes=[rtR[s_]])
                    S.add("dve", i_tt(r_[:, 1, :, :], x2, sinb, ALU.mult), reads=[qnR[s_], cR], writes=[rtR[s_]])
                    S.add("dve", i_tt(r_[:, 2, :, :], x2, cosb, ALU.mult), reads=[qnR[s_], cR], writes=[rtR[s_]])
                    S.add("dve", i_tt(r_[:, 3, :, :], x1, sinb, ALU.mult), reads=[qnR[s_], cR], writes=[rtR[s_]])

                def part2():
                    S.add("dve", i_tt(qb[qs][:, :, 0:16], r_[:, 0, :, :], r_[:, 1, :, :], ALU.subtract), reads=[rtR[s_], qbR[qs]], writes=[qbR[qs]])
                    S.add("dve", i_tt(qb[qs][:, :, 16:32], r_[:, 2, :, :], r_[:, 3, :, :], ALU.add), reads=[rtR[s_], qbR[qs]], writes=[qbR[qs]])
                return part1, part2

            def step(tile_A):
                recip = None
                newB = None
                if tile_A is not None:
                    recip, newB = tile_A()
                if pendB:
                    p1, p2 = pendB.pop(0)
                    if p1 is not None:
                        p1()
                else:
                    p2 = None
                if recip is not None:
                    recip()
                if p2 is not None:
                    p2()
                if len(pending) >= 3 or (tile_A is None and pending):
                    pending.pop(0)()
                if tile_A is not None:
                    pendB.append(newB if newB is not None else (None, None))

            for ci, cb in enumerate(cbs):
                if ci + 1 < len(cbs):
                    issue(ci + 1)
                wsl = ci % 2
                kind = cb // 2
                hd0 = (cb % 2) * 4
                for i in range(NT):
                    def tile_A(ci=ci, cb=cb, wsl=wsl, kind=kind, hd0=hd0, i=i):
                        nonlocal cnt
                        gtile = seg * NT + i
                        pb = cnt % 3
                        s_ = cnt % NS
                        qs = cnt % NQ
                        cnt += 1
                        for c in range(8):
                            S.add("pe", i_mm(ps[pb][:], self.hnT[:, c, i * 128:(i + 1) * 128], wq[wsl][:, c, :], c == 0, c == 7),
                                  reads=[self.hnTR[i], wqR[wsl]], writes=[psR[pb]])
                        if kind == 2:
                            vs = cnt % 2
                            S.add("act", i_acopy(vst[vs][:].rearrange("p h d -> p (h d)"), ps[pb][:]), reads=[psR[pb]], writes=[vstR[vs]])
                            S.add("sp", i_dma(self.Vs[hd0:hd0 + 4, gtile, :, :].rearrange("h p d -> p h d"), vst[vs][:]),
                                  reads=[vstR[vs]], key=vstR[vs])
                            return None, None
                        S.add("dve", i_memset(ssq[s_][:, 0, :], 0.0), writes=[ssR[s_]])
                        for hh in range(4):
                            S.add("act", i_act(sq[hh % 2][:], ps[pb][:, hh * 128:(hh + 1) * 128], AF.Square, accum_out=ssq[s_][:, 0, hh:hh + 1]),
                                  reads=[psR[pb], ssR[s_]], writes=[sqR[hh % 2], ssR[s_]])
                        S.add("act", i_act(ssq[s_][:, 1, :], ssq[s_][:, 0, :], AF.Sqrt, bias=128.0 * EPS, scale=1.0), reads=[ssR[s_]], writes=[ssR[s_]])

                        def recip():
                            S.add("dve", i_recip(ssq[s_][:, 1, :], ssq[s_][:, 1, :]), reads=[ssR[s_]], writes=[ssR[s_]])
                        pending.append(make_post(kind, hd0, i, qs))
                        return recip, make_B(kind, pb, s_, qs, gtile)
                    step(tile_A)
            while pendB or pending:
                step(None)
            S.end()

    def phase_attn(self):
        S, I = self.S, self.I
        with ExitStack() as es:
            KT = [self.sb(es, [128, 2 * T], BF16, "KT") for _ in range(2)]
            QT = [self.sb(es, [128, T], BF16, "QT") for _ in range(2)]
            V = [self.sb(es, [128, 2 * NT, 128], BF16, "V") for _ in range(2)]
            KTR = [S.res("KT0"), S.res("KT1")]
            QTR = [S.res("QT0"), S.res("QT1")]
            VR = [S.res("V0"), S.res("V1")]
            wo = self.sb(es, [128, 8, D], BF16, "wo")
            woR = [S.res("wo0"), S.res("wo1")]
            NPT = 4
            PT = [self.sb(es, [128, 512], BF16, "PT") for _ in range(NPT)]
            PTR = [S.res("PT%d" % i) for i in range(NPT)]
            maskT = [self.sb(es, [128, T], BF16, "maskT") for _ in range(2)]
            mTR = [S.res("maskT0"), S.res("maskT1")]
            km = self.sb(es, [128, 16], F32, "km")
            kmb = self.sb(es, [128, 16], BF16, "kmb")
            kmR = S.res("km")
            gm = self.sb(es, [128, 16, 16], F32, "gm")
            gmR = S.res("gm")
            mx = self.sb(es, [128, 16, 8], F32, "amx")
            mxR = S.res("amx")
            thr = self.sb(es, [128, 16], F32, "thr")
            thR = S.res("thr")
            sel = self.sb(es, [128, 16, 16], F32, "sel")
            selR = S.res("sel")
            mb = self.sb(es, [128, 256], BF16, "mb")
            mbR = S.res("mb")
            rsum = self.sb(es, [128, 256], F32, "rsum")
            rsR = S.res("rsum")
            ps, psR = self.ps, self.psR
            cR = self.constR
            oTR = self.hnTR

            for k in range(2):
                S.add("pool", i_dma(wo[:, 4 * k:4 * k + 4, :], I["wo"][:, 4 * k:4 * k + 4, :]), writes=[woR[k]], key=woR[k])
            self.eoh = self.sb(es, [128, 2048], BF16, "eoh")
            eohR = S.res("eoh")
            S.add("pool", i_dma(self.eoh[:], I["eoh"]), writes=[eohR], key=eohR)
            for k in range(2):
                S.add("dve", i_memset(maskT[k][:], 0.0), writes=[mTR[k]])

            def load(hd):
                sl = hd % 2
                S.add("sp", i_dma(KT[sl][:], self.Ks[hd]), writes=[KTR[sl]], key=KTR[sl])
                S.add("sp", i_dma(QT[sl][:], self.Qs[hd]), writes=[QTR[sl]], key=QTR[sl])
                S.add("sp", i_dma(V[sl][:], self.Vs[hd].rearrange("n p d -> p n d")), writes=[VR[sl]], key=VR[sl])

            def pro1(hd):
                sl = hd % 2
                S.add("dve", i_red(km[:], KT[sl][:].rearrange("p (n k) -> p n k", n=16), ALU.add), reads=[KTR[sl]], writes=[kmR])
                S.add("dve", i_ts(kmb[:], km[:], 1.0 / 256.0, None, ALU.mult), reads=[kmR], writes=[kmR])

            def pro2(hd):
                sl = hd % 2
                for i in range(NT):
                    S.add("pe", i_mm(ps[5][:, i * 16:(i + 1) * 16], QT[sl][:, i * 128:(i + 1) * 128], kmb[:], True, True),
                          reads=[QTR[sl], kmR], writes=[psR[5]])
                S.add("dve", i_tt(gm[:].rearrange("p a b -> p (a b)"), ps[5][:, 0:256], self.gbias[:], ALU.add),
                      reads=[psR[5], cR], writes=[gmR])
                for i in range(NT):
                    S.add("dve", i_max8(mx[:, i, :], gm[:, i, :]), reads=[gmR], writes=[mxR])
                S.add("dve", i_ts(thr[:], mx[:, :, 2], -1e29, None, ALU.max), reads=[mxR], writes=[thR])
                S.add("dve", i_tt(sel[:], gm[:], thr[:].unsqueeze(2).to_broadcast([128, 16, 16]), ALU.is_ge),
                      reads=[gmR, thR], writes=[selR])
                S.add("dve", i_ts(mb[:], sel[:].rearrange("p a b -> p (a b)"), -1.0, 1e5, ALU.add, ALU.mult), reads=[selR], writes=[mbR])

            def pro3(hd):
                sl = hd % 2
                for half in range(2):
                    for i8 in range(8):
                        i = half * 8 + i8
                        S.add("pe", i_tr(self.pst[half][0:16, i8 * 128:(i8 + 1) * 128], mb[:, i * 16:(i + 1) * 16], self.ident[:]),
                              reads=[mbR, cR], writes=[self.pstR[half]])
                    S.add("act", i_acopy(maskT[sl][0:16, half * 1024:(half + 1) * 1024], self.pst[half][0:16, :]),
                          reads=[self.pstR[half]], writes=[mTR[sl]])

            iters = [(hd, j, n) for hd in range(8) for j in range(8) for n in range(9 + j)]
            state = {}

            def partA(k):
                hd, j, n = iters[k]
                sl = hd % 2
                qcols = slice(j * 256, (j + 1) * 256)
                diag = (n == 8 + j)
                sb_ = k % 3
                for a in range(2):
                    S.add("pe", i_mm(ps[sb_][:, a * 256:(a + 1) * 256], KT[sl][:, n * 256 + a * 128: n * 256 + (a + 1) * 128],
                                      QT[sl][:, qcols], True, diag), reads=[KTR[sl], QTR[sl]], writes=[psR[sb_]])
                    if not diag:
                        S.add("pe", i_mm(ps[sb_][:, a * 256:(a + 1) * 256], self.eoh[:, n * 128:(n + 1) * 128],
                                          maskT[sl][:, qcols], False, True), reads=[eohR, mTR[sl]], writes=[psR[sb_]])

            def partB(k):
                hd, j, n = iters[k]
                sl = hd % 2
                qcols = slice(j * 256, (j + 1) * 256)
                diag = (n == 8 + j)
                sb_ = k % 3
                pt = k % NPT
                ob = 3 + (j % 2)
                S.add("act", i_act(PT[pt][:], ps[sb_][:], AF.Exp, bias=self.negB[:], scale=SCALE),
                      reads=[psR[sb_], cR], writes=[PTR[pt]])
                if diag:
                    S.add("dve", i_tt(PT[pt][:], PT[pt][:], self.dmask[:], ALU.mult), reads=[PTR[pt], cR], writes=[PTR[pt]])
                state.setdefault("pv", []).append((k, pt))

            def partC(k):
                hd, j, n = iters[k]
                sl = hd % 2
                qcols = slice(j * 256, (j + 1) * 256)
                diag = (n == 8 + j)
                pt = k % NPT
                ob = 3 + (j % 2)
                for a in range(2):
                    first = (n == 0 and a == 0)
                    last = (diag and a == 1)
                    S.add("pe", i_mm(ps[ob][:, 0:256], V[sl][:, 2 * n + a, :], PT[pt][:, a * 256:(a + 1) * 256], first, last, True),
                          reads=[VR[sl], PTR[pt]], writes=[psR[ob]])
                    S.add("pe", i_mm(ps[ob][:, 256:512], self.ones[:], PT[pt][:, a * 256:(a + 1) * 256], False, last, True),
                          reads=[cR, PTR[pt]], writes=[psR[ob]])
                if diag:
                    S.add("dve", i_recip(rsum[:], ps[ob][:, 256:512]), reads=[psR[ob]], writes=[rsR])
                    S.add("dve", i_tt(self.hnT[:, hd, qcols], ps[ob][:, 0:256], rsum[:], ALU.mult),
                          reads=[psR[ob], rsR], writes=[oTR[2 * j], oTR[2 * j + 1]])

            load(0)
            pro1(0)
            pro2(0)
            pro3(0)
            load(1)
            nit = len(iters)
            partA(0)
            for k in range(nit):
                hd, j, n = iters[k]
                partB(k)
                if k + 1 < nit:
                    hd2, j2, n2 = iters[k + 1]
                    if hd2 != hd and hd2 + 1 < 8:
                        pass
                    partA(k + 1)
                partC(k)
                if hd + 1 < 8 and n == 0:
                    if j == 1:
                        pro1(hd + 1)
                    elif j == 3:
                        pro2(hd + 1)
                    elif j == 5:
                        pro3(hd + 1)
                    elif j == 7 and hd + 2 < 8:
                        pass
                if n == 8 + j and j == 7 and hd + 2 < 8:
                    load(hd + 2)
            cnt = 0
            for i in range(NT):
                for hf in range(2):
                    b = cnt % 3
                    cnt += 1
                    for f in range(8):
                        S.add("pe", i_mm(ps[b][:], self.hnT[:, f, i * 128:(i + 1) * 128], wo[:, f, hf * 512:(hf + 1) * 512], f == 0, f == 7),
                              reads=[oTR[i], woR[f // 4]], writes=[psR[b]])
                    hsl = self.h[:, i, hf * 512:(hf + 1) * 512]
                    S.add("dve", i_tt(hsl, ps[b][:], hsl, ALU.add), reads=[psR[b], self.hR[i]], writes=[self.hR[i]])
            S.end()

    def router_sparse(self, es, wr, wrR, hn, hnR):
        S = self.S
        ps, psR = self.ps, self.psR
        cR = self.constR
        lg = self.sb(es, [128, NT, 8], F32, "lg")
        mx = self.sb(es, [128, NT, 8], F32, "mx")
        tmp = self.sb(es, [128, 3, NT], F32, "rtmp")
        eq = self.sb(es, [128, 2, NT, 8], F32, "eq")
        selb = self.sb(es, [128, NT, 8], BF16, "selb")
        cum = self.sb(es, [128, NT, 8], F32, "cum")
        tot = self.sb(es, [128, 8], F32, "tot")
        ng = self.sb(es, [128, 2, 8], F32, "ng")
        off = self.sb(es, [128, 2, 8], F32, "off")
        slotv = self.sb(es, [128, NT, 8], F32, "slotv")
        t3 = self.sb(es, [128, NT, 8], F32, "t3")
        slotf = self.sb(es, [128, 2, NT], F32, "slotf")
        cmp_ = self.sb(es, [128, NGRP, 8], F32, "cmp")
        gef = self.sb(es, [128, NGRP], F32, "gef")
        iwf = self.sb(es, [128, NGRP, 28], F32, "iwf")
        idf = self.sb(es, [128, NGRP, 14], F32, "idf")
        lgR, mxR, tR, eR, sbR, cuR, toR, ngR, ofR, svR, t3R, sfR, cmR, geR, iwR, idR = [S.res("rs_%d" % i) for i in range(16)]
        rR = self.routeR
        psr = ps[0]
        for i in range(NT):
            for c in range(8):
                S.add("pe", i_mm(psr[:, i * 8:(i + 1) * 8], self.hnT[:, c, i * 128:(i + 1) * 128], wr[:, c, :],
                                  c == 0, c == 7), reads=[self.hnTR[i], wrR], writes=[psR[0]])
        S.add("dve", i_copy(lg[:].rearrange("p t e -> p (t e)"), psr[:, 0:NT * 8]), reads=[psR[0]], writes=[lgR])
        for i in range(NT):
            S.add("dve", i_max8(mx[:, i, :], lg[:, i, :]), reads=[lgR], writes=[mxR])
        S.add("dve", i_tt(tmp[:, 0, :], mx[:, :, 1], mx[:, :, 0], ALU.subtract), reads=[mxR], writes=[tR])
        S.add("act", i_act(tmp[:, 1, :], tmp[:, 0, :], AF.Exp), reads=[tR], writes=[tR])
        S.add("dve", i_ts(tmp[:, 2, :], tmp[:, 1, :], 1.0, None, ALU.add), reads=[tR], writes=[tR])
        S.add("dve", i_recip(self.p12[:, 0, :], tmp[:, 2, :]), reads=[tR], writes=[rR])
        S.add("dve", i_tt(self.p12[:, 1, :], tmp[:, 1, :], self.p12[:, 0, :], ALU.mult), reads=[tR, rR], writes=[rR])
        for k in range(2):
            S.add("dve", i_tt(eq[:, k, :, :], lg[:], mx[:, :, k:k + 1].to_broadcast([128, NT, 8]), ALU.is_equal),
                  reads=[lgR, mxR], writes=[eR])
        S.add("dve", i_tt(selb[:], eq[:, 0, :, :], eq[:, 1, :, :], ALU.add), reads=[eR], writes=[sbR])
        for i in range(NT):
            for i2 in range(i):
                S.add("pe", i_mm(ps[1][:, i * 8:(i + 1) * 8], self.ones[:], selb[:, i2, :], i2 == 0, False),
                      reads=[sbR, cR], writes=[psR[1]])
            S.add("pe", i_mm(ps[1][:, i * 8:(i + 1) * 8], self.trilb[:], selb[:, i, :], i == 0, True),
                  reads=[sbR, cR], writes=[psR[1]])
        for i2 in range(NT):
            S.add("pe", i_mm(ps[2][:, 0:8], self.ones[:], selb[:, i2, :], i2 == 0, i2 == NT - 1),
                  reads=[sbR, cR], writes=[psR[2]])
        S.add("dve", i_copy(cum[:].rearrange("p t e -> p (t e)"), ps[1][:, 0:NT * 8]), reads=[psR[1]], writes=[cuR])
        S.add("dve", i_copy(tot[:], ps[2][:, 0:8]), reads=[psR[2]], writes=[toR])
        S.add("dve", i_ts(ng[:, 0, :], tot[:], 0.5, None, ALU.is_gt), reads=[toR], writes=[ngR])
        for thr_ in (512.5, 1024.5, 1536.5):
            S.add("dve", i_ts(ng[:, 1, :], tot[:], thr_, None, ALU.is_gt), reads=[toR, ngR], writes=[ngR])
            S.add("dve", i_tt(ng[:, 0, :], ng[:, 0, :], ng[:, 1, :], ALU.add), reads=[ngR], writes=[ngR])
        S.add("dve", i_memset(off[:], 0.0), writes=[ofR])
        for e in range(1, 8):
            S.add("dve", i_tt(off[:, 0, e:e + 1], off[:, 0, e - 1:e], ng[:, 0, e - 1:e], ALU.add), reads=[ofR, ngR], writes=[ofR])
        S.add("dve", i_ts(off[:, 1, :], off[:, 0, :], 512.0, -1.0, ALU.mult, ALU.add), reads=[ofR], writes=[ofR])
        S.add("dve", i_tt(slotv[:], cum[:], off[:, 1, :].unsqueeze(1).to_broadcast([128, NT, 8]), ALU.add),
              reads=[cuR, ofR], writes=[svR])
        for k in range(2):
            S.add("dve", i_tt(t3[:], eq[:, k, :, :], slotv[:], ALU.mult), reads=[eR, svR, t3R], writes=[t3R])
            S.add("dve", i_red(slotf[:, k, :], t3[:], ALU.add), reads=[t3R], writes=[sfR])
        S.add("dve", i_copy(self.slotu[:], slotf[:]), reads=[sfR], writes=[rR])
        S.add("dve", i_tt(cmp_[:], off[:, 0, :].unsqueeze(1).to_broadcast([128, NGRP, 8]),
                          self.giota[:].unsqueeze(2).to_broadcast([128, NGRP, 8]), ALU.is_le), reads=[ofR, cR], writes=[cmR])
        S.add("dve", i_red(gef[:], cmp_[:], ALU.add), reads=[cmR], writes=[geR])
        S.add("dve", i_ts(gef[:], gef[:], -1.0, None, ALU.add), reads=[geR], writes=[geR])
        S.add("dve", i_stt(iwf[:], gef[:].unsqueeze(2).to_broadcast([128, NGRP, 28]), 3584.0,
                           self.basew[:].unsqueeze(1).to_broadcast([128, NGRP, 28]), ALU.mult, ALU.add),
              reads=[geR, cR], writes=[iwR])
        S.add("dve", i_stt(idf[:], gef[:].unsqueeze(2).to_broadcast([128, NGRP, 14]), 1792.0,
                           self.based[:].unsqueeze(1).to_broadcast([128, NGRP, 14]), ALU.mult, ALU.add),
              reads=[geR, cR], writes=[idR])
        S.add("dve", i_copy(self.idxw[:], iwf[:]), reads=[iwR], writes=[rR])
        S.add("dve", i_copy(self.idxd[:], idf[:]), reads=[idR], writes=[rR])
        for i in range(NT):
            for k in range(2):
                S.add("pool", i_scatter(self.xgD, self.slotu[:, k, i:i + 1], hn[i], NSLOT - 1),
                      reads=[hnR[i], rR], key=hnR[i])

    def phase_moe_sparse(self):
        S, I = self.S, self.I
        mgu2, mdn2 = I["mgu"], I["mdn"]
        with ExitStack() as es:
            wgu = [self.sb(es, [128, 8, 2, 512], BF16, "wgu") for _ in range(2)]
            wdn = [self.sb(es, [128, 4, D], BF16, "wdn") for _ in range(2)]
            wguR = [[S.res("wgu%d_%d" % (s_, q)) for q in range(4)] for s_ in range(2)]
            wdnR = [[S.res("wdn%d_%d" % (s_, q)) for q in range(2)] for s_ in range(2)]
            Y = [self.sb(es, [128, 4, 512], BF16, "Y") for _ in range(2)]
            YR = [[S.res("Y%d_%d" % (s_, f)) for f in range(4)] for s_ in range(2)]
            sg = [self.sb(es, [128, 512], F32, "sg") for _ in range(2)]
            sgR = [S.res("sg0"), S.res("sg1")]
            xtm = [self.sb(es, [128, D], BF16, "xtm") for _ in range(2)]
            xtmR = [S.res("xtm0"), S.res("xtm1")]
            xgT = [self.sb(es, [128, 8, 512], BF16, "xgT") for _ in range(2)]
            xgTR = [[S.res("xgT%d_%d" % (s_, t)) for t in range(4)] for s_ in range(2)]
            yacc = self.sb(es, [128, 4, D], F32, "yacc")
            yaccR = [S.res("yacc%d" % t) for t in range(4)]
            ps, psR = self.ps, self.psR
            rR = self.routeR
            yDR = S.res("yD")

            def issue_w(g, fb):
                sl = (g * 7 + fb) % 2
                wf = wgu[sl][:].rearrange("p k g f -> p (k g f)")
                for q in range(4):
                    S.add("pool", i_gather(wf[:, q * 2048:(q + 1) * 2048], mgu2, self.idxw[:, g, fb * 4 + q: fb * 4 + q + 1],
                                           NE * 128 * 28 - 1), reads=[rR], writes=[wguR[sl][q]], key=wguR[sl][q])
                df = wdn[sl][:].rearrange("p c n -> p (c n)")
                for q in range(2):
                    S.add("pool", i_gather(df[:, q * 2048:(q + 1) * 2048], mdn2, self.idxd[:, g, fb * 2 + q: fb * 2 + q + 1],
                                           NE * 128 * 14 - 1), reads=[rR], writes=[wdnR[sl][q]], key=wdnR[sl][q])

            def load_x(g):
                sl = g % 2
                for ti in range(4):
                    st_ = g * 4 + ti
                    xs = st_ % 2
                    S.add("sp", i_dma(xtm[xs][:], self.xgD[st_ * 128:(st_ + 1) * 128, :]), writes=[xtmR[xs]], key=xtmR[xs])
                    for c in range(8):
                        S.add("pe", i_tr(self.pst[xs][:, c * 128:(c + 1) * 128], xtm[xs][:, c * 128:(c + 1) * 128], self.ident[:]),
                              reads=[xtmR[xs]], writes=[self.pstR[xs]])
                    S.add("act", i_acopy(xgT[sl][:, :, ti * 128:(ti + 1) * 128],
                                          self.pst[xs][:].rearrange("p (c t) -> p c t", c=8)),
                          reads=[self.pstR[xs]], writes=[xgTR[sl][ti]])

            issue_w(0, 0)
            load_x(0)
            cnt = 0
            dcnt = 0
            for g in range(NGRP):
                xsl = g % 2
                xr = xgTR[xsl]
                for fb in range(7):
                    if fb < 6:
                        issue_w(g, fb + 1)
                    elif g + 1 < NGRP:
                        issue_w(g + 1, 0)
                    if fb == 3 and g + 1 < NGRP:
                        load_x(g + 1)
                    sl = (g * 7 + fb) % 2
                    ysl = (g * 7 + fb) % 2
                    for fc in range(4):
                        pb = cnt % 2
                        cnt += 1
                        for c in range(8):
                            S.add("pe", i_mm(ps[pb][:], wgu[sl][:, c, 0, fc * 128:(fc + 1) * 128], xgT[xsl][:, c, :],
                                              c == 0, c == 7), reads=xr + [wguR[sl][c // 2]], writes=[psR[pb]])
                        for c in range(8):
                            S.add("pe", i_mm(ps[2 + pb][:], wgu[sl][:, c, 1, fc * 128:(fc + 1) * 128], xgT[xsl][:, c, :],
                                              c == 0, c == 7), reads=xr + [wguR[sl][c // 2]], writes=[psR[2 + pb]])
                        S.add("act", i_act(sg[pb][:], ps[pb][:], AF.Silu), reads=[psR[pb]], writes=[sgR[pb]])
                        S.add("dve", i_tt(Y[ysl][:, fc, :], sg[pb][:], ps[2 + pb][:], ALU.mult),
                              reads=[sgR[pb], psR[2 + pb]], writes=[YR[ysl][fc]])
                    for ti in range(4):
                        for hf in range(2):
                            db = 4 + dcnt % 2
                            dcnt += 1
                            for fc in range(4):
                                S.add("pe", i_mm(ps[db][:], Y[ysl][:, fc, ti * 128:(ti + 1) * 128],
                                                  wdn[sl][:, fc, hf * 512:(hf + 1) * 512], fc == 0, fc == 3),
                                      reads=[YR[ysl][fc], wdnR[sl][fc // 2]], writes=[psR[db]])
                            ysl_ = yacc[:, ti, hf * 512:(hf + 1) * 512]
                            if fb == 0:
                                S.add("act", i_acopy(ysl_, ps[db][:]), reads=[psR[db]], writes=[yaccR[ti]])
                            else:
                                S.add("dve", i_tt(ysl_, ps[db][:], ysl_, ALU.add), reads=[psR[db], yaccR[ti]], writes=[yaccR[ti]])
                S.add("sp", i_dma(self.yD[g * 512:(g + 1) * 512, :].rearrange("(a p) c -> p a c", p=128), yacc[:]),
                      reads=yaccR, writes=[yDR], key=yaccR[0])
            self.phase_combine(inline_es=es, yDR=yDR)

    def phase_combine(self, inline_es=None, yDR=None):
        S = self.S
        yd = [yDR] if yDR is not None else []
        with (ExitStack() if inline_es is None else _Borrow(inline_es)) as es:
            ya = [self.sb(es, [128, D], F32, "ya") for _ in range(2)]
            yb = [self.sb(es, [128, D], F32, "yb") for _ in range(2)]
            yaR = [S.res("ya0"), S.res("ya1")]
            ybR = [S.res("yb0"), S.res("yb1")]
            rR = self.routeR
            outs = []
            for i in range(NT):
                s_ = i % 2
                S.add("pool", i_gather(ya[s_][:], self.yD, self.slotu[:, 0, i:i + 1], NSLOT - 1), reads=[rR] + yd, writes=[yaR[s_]], key=yaR[s_])
                S.add("pool", i_gather(yb[s_][:], self.yD, self.slotu[:, 1, i:i + 1], NSLOT - 1), reads=[rR] + yd, writes=[ybR[s_]], key=ybR[s_])
                S.add("dve", i_stt(self.h[:, i, :], ya[s_][:], self.p12[:, 0, i:i + 1], self.h[:, i, :], ALU.mult, ALU.add),
                      reads=[yaR[s_], rR, self.hR[i]], writes=[self.hR[i]])
                S.add("dve", i_stt(self.h[:, i, :], yb[s_][:], self.p12[:, 1, i:i + 1], self.h[:, i, :], ALU.mult, ALU.add),
                      reads=[ybR[s_], rR, self.hR[i]], writes=[self.hR[i]])
                outs.append(S.add("sp", i_dma(self.out[i], self.h[:, i, :]), reads=[self.hR[i]], key=self.hR[i]))
            S.end(extra_wait=outs)

    def phase_store(self):
        S = self.S
        outs = []
        for i in range(NT):
            outs.append(S.add("sp", i_dma(self.out[i], self.h[:, i, :]), reads=[self.hR[i]], key=self.hR[i]))
        S.end(extra_wait=outs)


def _prep_gu(Wg, Wu, widths):
    parts = []
    f0 = 0
    for w in widths:
        blk = np.stack([Wg[:, f0:f0 + w], Wu[:, f0:f0 + w]], axis=1)
        blk = blk.reshape(8, 128, 2, w).transpose(1, 0, 2, 3).reshape(128, 16 * w)
        parts.append(blk)
        f0 += w
    return np.ascontiguousarray(np.concatenate(parts, axis=1))


def _prep_dn(Wd):
    nch = Wd.shape[0] // 128
    return np.ascontiguousarray(Wd.reshape(nch, 128, D).transpose(1, 0, 2).reshape(128, nch * D))


def _pk(W):
    return np.ascontiguousarray(W.reshape(8, 128, W.shape[1]).transpose(1, 0, 2))


def _rep(v, n=128):
    return np.ascontiguousarray(np.broadcast_to(np.asarray(v, np.float32)[None, :], (n, v.shape[0])))


def _rope_tables(pos0):
    half = 16
    inv_freq = (np.float32(500000.0) ** (-np.arange(half, dtype=np.float32) / np.float32(half))).astype(np.float32)
    pos = (pos0 + np.arange(T)).astype(np.float32)
    ang = (pos[:, None] * inv_freq[None, :]).astype(np.float32)
    return np.cos(ang).astype(np.float32), np.sin(ang).astype(np.float32)


_CACHE = {}


def _shared_inputs(inp, used):
    f = lambda k: np.asarray(inp[k], np.float32)
    sh = {}
    sh["gn"] = np.stack([_rep(f("e_mix_norm")[0]), _rep(f("e_ffn_norm")[0]), _rep(f("o_mix_norm")[0]), _rep(f("o_ffn_norm")[0])])
    sh["win"] = _pk(f("e_w_in")[0])
    sh["wout"] = _pk(f("e_w_out")[0])
    cw = f("e_conv_w")[0]
    sh["convw"] = np.ascontiguousarray(cw.reshape(3, 4, 128).transpose(2, 1, 0).reshape(128, 12))
    sh["lng"] = np.ascontiguousarray(f("e_gmlp_ln_g")[0].reshape(4, 128).T)
    sh["lnb"] = _rep(f("e_gmlp_ln_b")[0])
    sh["wsT"] = np.ascontiguousarray(f("e_w_spatial")[0].transpose(2, 0, 1))
    bs = f("e_b_spatial")[0]
    bsp = np.zeros((128, 4, 128), np.float32)
    for j in range(4):
        for hf in range(2):
            bsp[hf * 64:(hf + 1) * 64, j, :] = bs[2 * j + hf][None, :]
    sh["bsp"] = bsp.reshape(128, 512)
    sh["tril"] = np.triu(np.ones((128, 128), np.float32))
    sh["egu"] = _prep_gu(f("e_w_gate")[0], f("e_w_up")[0], L0_BLOCKS)
    sh["edn"] = _prep_dn(f("e_w_down")[0])
    sh["wqkv"] = _pk(f("o_w_qkv")[0])
    sh["gq"] = _rep(f("o_q_norm")[0])
    sh["gk"] = _rep(f("o_k_norm")[0])
    sh["wo"] = _pk(f("o_w_o")[0])
    sh["wr"] = _pk(f("o_w_router")[0])
    if "mgu" in used:
        wg, wu, wd = f("o_w_gate")[0], f("o_w_up")[0], f("o_w_down")[0]
        sh["mgu"] = np.stack([_prep_gu(wg[e], wu[e], MOE_BLOCKS) for e in range(NE)])
        sh["mdn"] = np.stack([_prep_dn(wd[e]) for e in range(NE)])
        if SPARSE:
            sh["mgu"] = sh["mgu"].reshape(NE * 128 * 28, 2048)
            sh["mdn"] = sh["mdn"].reshape(NE * 128 * 14, 2048)
    sh["trilb"] = np.triu(np.ones((128, 128), np.float32))
    sh["giota"] = np.ascontiguousarray(np.broadcast_to(np.arange(NGRP, dtype=np.float32)[None, :], (128, NGRP)))
    sh["basew"] = (np.arange(128, dtype=np.float32)[:, None] * 28 + np.arange(28, dtype=np.float32)[None, :]).astype(np.float32)
    sh["based"] = (np.arange(128, dtype=np.float32)[:, None] * 14 + np.arange(14, dtype=np.float32)[None, :]).astype(np.float32)
    sh["ident"] = np.eye(128, dtype=np.float32)
    k = np.arange(128)[:, None]
    q = np.arange(256)[None, :]
    dm = np.concatenate([(k <= q), (128 + k <= q)], axis=1).astype(np.float32)
    sh["dmask"] = dm
    eoh = np.zeros((128, 16, 128), np.float32)
    for n in range(16):
        eoh[n, n, :] = 1.0
    sh["eoh"] = eoh.reshape(128, 2048)
    return sh


def _core_inputs(x, b, half, sh):
    m = dict(sh)
    m["xo"] = np.ascontiguousarray(x[b, half * T:(half + 1) * T].reshape(NT, 128, D))
    if half == 1:
        m["xp"] = np.ascontiguousarray(x[b, 0:T].reshape(NT, 128, D))
    else:
        m["xp"] = np.zeros((NT, 128, D), np.float32)
    cp, sp_ = _rope_tables(0)
    co, so = _rope_tables(half * T)
    rope = np.zeros((128, 2, 32, 16), np.float32)
    rope[:, 0, 0:16, :] = cp.reshape(16, 128, 16).transpose(1, 0, 2)
    rope[:, 1, 0:16, :] = sp_.reshape(16, 128, 16).transpose(1, 0, 2)
    rope[:, 0, 16:32, :] = co.reshape(16, 128, 16).transpose(1, 0, 2)
    rope[:, 1, 16:32, :] = so.reshape(16, 128, 16).transpose(1, 0, 2)
    m["rope"] = rope
    gb = np.full((16, 16), -1e30, np.float32)
    for i in range(16):
        j = i // 2
        if half == 1:
            gb[i, 0:8] = 0.0
        gb[i, 8:8 + j] = 0.0
    m["gbias"] = np.ascontiguousarray(np.broadcast_to(gb.reshape(1, 256), (128, 256)))
    return m


def _run(inputs, stop_after=None):
    x = np.asarray(inputs["x"], np.float32)
    key = stop_after
    if key not in _CACHE:
        bld = Builder(stop_after)
        _CACHE[key] = (bld.build(), set(bld.I.keys()))
    nc, used = _CACHE[key]
    sh = _shared_inputs(inputs, used)
    in_maps = []
    for c in range(8):
        m = _core_inputs(x, c // 2, c % 2, sh)
        in_maps.append({k: v for k, v in m.items() if k in used})
    res = run_bass_kernel_spmd(nc, in_maps, core_ids=list(range(8)))
    out = np.zeros((4, 4096, D), np.float32)
    for c in range(8):
        b, half = c // 2, c % 2
        out[b, half * T:(half + 1) * T] = np.asarray(res.results[c]["out"]).reshape(T, D)
    return out


def kernel(**inputs):
    return _run(inputs)
```
